# Optimizing a Trainium2 kernel written in Bass

```python
import jax, jax.numpy as jnp
from jax import lax
import numpy as np

D_MODEL = 1024
BATCH = 8
SEQ = 2048
DEPTH = 2

D_MIX = D_MODEL
CONV_CH = D_MIX // 2
SGU_CH = D_MIX - CONV_CH
CONV_GROUPS = 8
SGU_HEADS = 4
SGU_HEAD_DIM = SGU_CH // SGU_HEADS
CONV_WIDTH = 31
CHUNK = 128
D_IN = 2 * CONV_CH + 2 * SGU_CH
D_FF = 2816
N_EXPERTS = 8
TOP_K = 2
N_DENSE = (DEPTH + 1) // 2
N_MOE = DEPTH // 2
EPS = 1e-6

kernel_name = "hybrid_conv_gmlp_moe_block"


def rms_norm(x, g):
    xf = x.astype(jnp.float32)
    y = xf * lax.rsqrt(jnp.mean(xf * xf, axis=-1, keepdims=True) + EPS)
    return (y * g.astype(jnp.float32)).astype(x.dtype)


def group_layer_norm(x, n_groups, g, b):
    shp = x.shape
    xf = x.astype(jnp.float32).reshape(shp[:-1] + (n_groups, shp[-1] // n_groups))
    mu = jnp.mean(xf, axis=-1, keepdims=True)
    var = jnp.mean(jnp.square(xf - mu), axis=-1, keepdims=True)
    y = ((xf - mu) * lax.rsqrt(var + EPS)).reshape(shp)
    return (y * g.astype(jnp.float32) + b.astype(jnp.float32)).astype(x.dtype)


def causal_depthwise_conv(c, w, b):
    y = lax.conv_general_dilated(
        c, w, window_strides=(1,), padding=[(CONV_WIDTH - 1, 0)],
        dimension_numbers=('NWC', 'WIO', 'NWC'), feature_group_count=c.shape[-1])
    return y + b


def hybrid_mixer(h, w_in, conv_w, conv_b, conv_ng, conv_nb, sgu_ng, sgu_nb, sgu_w, sgu_b, w_out):
    B, S, _ = h.shape
    z = jnp.einsum('bsd,de->bse', h, w_in)
    a, gate, u, v = jnp.split(z, [CONV_CH, 2 * CONV_CH, 2 * CONV_CH + SGU_CH], axis=-1)
    c = a * jax.nn.sigmoid(gate)
    c = causal_depthwise_conv(c, conv_w, conv_b)
    c = jax.nn.silu(group_layer_norm(c, CONV_GROUPS, conv_ng, conv_nb))
    u = jax.nn.gelu(u, approximate=False)
    v = group_layer_norm(jax.nn.gelu(v, approximate=False), SGU_HEADS, sgu_ng, sgu_nb)
    n_chunks = S // CHUNK
    vc = v.reshape(B, n_chunks, CHUNK, SGU_HEADS, SGU_HEAD_DIM)
    mask = jnp.tril(jnp.ones((CHUNK, CHUNK), dtype=bool))
    ws = jnp.where(mask[None], sgu_w, jnp.zeros((), sgu_w.dtype))
    sp = jnp.einsum('hts,bnshd->bnthd', ws, vc) + jnp.transpose(sgu_b)[None, None, :, :, None]
    g_out = u * sp.reshape(B, S, SGU_CH)
    y = jnp.concatenate([c, g_out], axis=-1)
    return jnp.einsum('bse,ed->bsd', y, w_out)


def swiglu(h, wg, wu, wd):
    return jnp.einsum('...f,fd->...d', jax.nn.silu(h @ wg) * (h @ wu), wd)


def moe_swiglu(h, router, wg, wu, wd):
    B, S, D = h.shape
    ht = h.reshape(B * S, D)
    logits = jnp.einsum('td,de->te', ht, router).astype(jnp.float32)
    top_vals, top_idx = lax.top_k(logits, TOP_K)
    gates = jax.nn.softmax(top_vals, axis=-1)
    dense_gate = jnp.sum(jax.nn.one_hot(top_idx, N_EXPERTS, dtype=jnp.float32) * gates[..., None], axis=1)
    dense_gate = dense_gate.astype(h.dtype)
    out = jnp.zeros_like(ht)
    for e in range(N_EXPERTS):
        out = out + dense_gate[:, e:e + 1] * swiglu(ht, wg[e], wu[e], wd[e])
    return out.reshape(B, S, D)


def setup_inputs(seed: int = 0) -> dict:
    key = jax.random.key(seed)
    ks = jax.random.split(key, 24)
    nrm = lambda k, shp, s: jax.random.normal(k, shp, jnp.float32) * s
    L = DEPTH
    return {
        "x": nrm(ks[0], (BATCH, SEQ, D_MODEL), 1.0),
        "norm_mix": 1.0 + nrm(ks[1], (L, D_MODEL), 0.05),
        "w_in": nrm(ks[2], (L, D_MODEL, D_IN), D_MODEL ** -0.5),
        "conv_w": nrm(ks[3], (L, CONV_WIDTH, 1, CONV_CH), CONV_WIDTH ** -0.5),
        "conv_b": nrm(ks[4], (L, CONV_CH), 0.02),
        "conv_ng": 1.0 + nrm(ks[5], (L, CONV_CH), 0.05),
        "conv_nb": nrm(ks[6], (L, CONV_CH), 0.02),
        "sgu_ng": 1.0 + nrm(ks[7], (L, SGU_CH), 0.05),
        "sgu_nb": nrm(ks[8], (L, SGU_CH), 0.02),
        "sgu_w": nrm(ks[9], (L, SGU_HEADS, CHUNK, CHUNK), CHUNK ** -0.5),
        "sgu_b": 1.0 + nrm(ks[10], (L, SGU_HEADS, CHUNK), 0.1),
        "w_out": nrm(ks[11], (L, D_MIX, D_MODEL), D_MIX ** -0.5),
        "norm_ffn": 1.0 + nrm(ks[12], (L, D_MODEL), 0.05),
        "ffn_wg": nrm(ks[13], (N_DENSE, D_MODEL, D_FF), D_MODEL ** -0.5),
        "ffn_wu": nrm(ks[14], (N_DENSE, D_MODEL, D_FF), D_MODEL ** -0.5),
        "ffn_wd": nrm(ks[15], (N_DENSE, D_FF, D_MODEL), D_FF ** -0.5),
        "moe_router": nrm(ks[16], (N_MOE, D_MODEL, N_EXPERTS), D_MODEL ** -0.5),
        "moe_wg": nrm(ks[17], (N_MOE, N_EXPERTS, D_MODEL, D_FF), D_MODEL ** -0.5),
        "moe_wu": nrm(ks[18], (N_MOE, N_EXPERTS, D_MODEL, D_FF), D_MODEL ** -0.5),
        "moe_wd": nrm(ks[19], (N_MOE, N_EXPERTS, D_FF, D_MODEL), D_FF ** -0.5),
        "norm_final": 1.0 + nrm(ks[20], (D_MODEL,), 0.05),
    }


def reference(x, norm_mix, w_in, conv_w, conv_b, conv_ng, conv_nb, sgu_ng, sgu_nb, sgu_w, sgu_b,
              w_out, norm_ffn, ffn_wg, ffn_wu, ffn_wd, moe_router, moe_wg, moe_wu, moe_wd, norm_final):
    for l in range(DEPTH):
        h = rms_norm(x, norm_mix[l])
        x = x + hybrid_mixer(h, w_in[l], conv_w[l], conv_b[l], conv_ng[l], conv_nb[l],
                             sgu_ng[l], sgu_nb[l], sgu_w[l], sgu_b[l], w_out[l])
        h = rms_norm(x, norm_ffn[l])
        i = l // 2
        if l % 2 == 0:
            x = x + swiglu(h, ffn_wg[i], ffn_wu[i], ffn_wd[i])
        else:
            x = x + moe_swiglu(h, moe_router[i], moe_wg[i], moe_wu[i], moe_wd[i])
    return rms_norm(x, norm_final)
```

```python
import numpy as np
from contextlib import ExitStack

import concourse.bass as bass
import concourse.mybir as mybir
from concourse.bass_utils import run_bass_kernel_spmd

F32 = mybir.dt.float32
BF16 = mybir.dt.bfloat16
AF = mybir.ActivationFunctionType
ALU = mybir.AluOpType
AX = mybir.AxisListType

D = 1024
S = 2048
L = 2
KC = 8
TG = 4
TW = 512
DFF = 2816
NFC = 22
NE = 8
CW = 31
EPS = 1e-6
FBLOCKS = [(0, 8), (8, 8), (16, 6)]
NSLOT = 8
NTMP = 12
TSPLIT = ([0], [1], [2], [3])

V_PER_L = 8 + 8 + 4 + 4 + 4 + 4 * CW
def v_nmix(l): return l * V_PER_L
def v_nffn(l): return l * V_PER_L + 8
def v_cb(l): return l * V_PER_L + 16
def v_cng(l): return l * V_PER_L + 20
def v_cnb(l): return l * V_PER_L + 24
def v_cw(l): return l * V_PER_L + 28
V_NFIN = L * V_PER_L
NV = V_NFIN + 8

SAME_ENGINE_SYNC = True


class Buf:
    __slots__ = ("w", "r", "name")

    def __init__(self, name=""):
        self.w = None
        self.r = []
        self.name = name


class Sched:
    ENGS = ("pe", "act", "dve", "pool", "sp")

    def __init__(self, dry=False):
        self.dry = dry
        self.q = {e: [] for e in self.ENGS}
        self.cnt = {e: 0 for e in self.ENGS}
        self.seen = {e: {} for e in self.ENGS}
        self.dma_cnt = {}

    def op(self, eng, fn, reads=(), writes=(), dma_key=None, war=()):
        if self.dry:
            return None
        deps = {}
        def add(ev):
            if ev is None:
                return
            k, v = ev
            if deps.get(k, 0) < v:
                deps[k] = v
        for b in reads:
            add(b.w)
        for b in list(writes) + list(war):
            add(b.w)
            for ev in b.r:
                add(ev)
        waits = []
        for k, v in deps.items():
            if k == eng and dma_key is None and not SAME_ENGINE_SYNC:
                continue
            if self.seen[eng].get(k, 0) >= v:
                continue
            self.seen[eng][k] = v
            waits.append((k, v))
        if dma_key is not None:
            key = ("dma", dma_key)
            self.dma_cnt[key] = self.dma_cnt.get(key, 0) + 16
            ev = (key, self.dma_cnt[key])
        else:
            self.cnt[eng] += 1
            ev = (eng, self.cnt[eng])
        self.q[eng].append((waits, fn, ev))
        for b in reads:
            b.r.append(ev)
        for b in writes:
            b.w = ev
            b.r = []
        return ev


def build_program(stop_after=None):
    nc = bass.Bass("TRN2", target_bir_lowering=False)
    dram = {}
    def din(name, shape):
        dram[name] = nc.dram_tensor(name, list(shape), F32, kind="ExternalInput").ap()
        return dram[name]
    xT_d = din("xT", [128, KC, S])
    vecs_d = din("vecs", [128, NV])
    sgu_gb_d = din("sgu_gb", [128, L, 2, 512])
    sgu_bb_d = din("sgu_bb", [128, L, 4, 128])
    sgu_wT_d = din("sgu_wT", [128, L, 4, 128])
    consts_d = din("consts", [128, 2, 128])
    router_d = din("router", [128, KC, NE])
    w_in_d = din("w_in", [L, D, 2 * D])
    w_out_d = din("w_out", [L, D, D])
    ffn_wg_d = din("ffn_wg", [1, D, DFF])
    ffn_wu_d = din("ffn_wu", [1, D, DFF])
    ffn_wd_d = din("ffn_wd", [1, DFF, D])
    moe_wg_d = din("moe_wg", [1, NE, D, DFF])
    moe_wu_d = din("moe_wu", [1, NE, D, DFF])
    moe_wd_d = din("moe_wd", [1, NE, DFF, D])
    yT_d = nc.dram_tensor("yT", [128, KC, S], F32, kind="ExternalOutput").ap()

    es = ExitStack()
    with es:
        def sb(name, shape, dt):
            return es.enter_context(nc.sbuf_tensor(name, list(shape), dt))
        xT = sb("xT_sb", [128, KC, S], F32)
        hT = sb("hT_sb", [128, KC, S], BF16)
        REG = 8 * S + 4096
        reg = sb("reg_sb", [128, REG], BF16)
        ring_all = sb("ring", [128, NSLOT, 8, 128], BF16)
        ring = [ring_all[:, i, :, :] for i in range(NSLOT)]
        tmps = [sb(f"tmp{i}", [128, TW], F32) for i in range(NTMP)]
        vecs = sb("vecs_sb", [128, NV], F32)
        sgu_gb = sb("sgu_gb_sb", [128, 2, 512], F32)
        sgu_bb = sb("sgu_bb_sb", [128, 4, 128], F32)
        sgu_wTf = sb("sgu_wTf_sb", [128, 4, 128], F32)
        sgu_wTb = sb("sgu_wTb_sb", [128, 4, 128], BF16)
        consts = sb("consts_sb", [128, 2, 128], F32)
        identb = sb("identb_sb", [128, 128], BF16)
        ones_m = sb("ones_m_sb", [128, 128], F32)
        ones_1 = sb("ones_1_sb", [128, 128], F32)
        blk_m = sb("blk_m_sb", [128, 128], F32)
        router = sb("router_sb", [128, KC, NE], F32)
        eps_t = sb("eps_sb", [128, 1], F32)
        rg = sb("rg_sb", [128, KC, NE], F32)
        Ltok = sb("Ltok_sb", [128, 16, NE], F32)
        L2tok = sb("L2tok_sb", [128, 16, NE], F32)
        eq1 = sb("eq1_sb", [128, 16, NE], F32)
        eq2 = sb("eq2_sb", [128, 16, NE], F32)
        dg = sb("dg_sb", [128, 16, NE], F32)
        m1 = sb("m1_sb", [128, 16], F32)
        m2 = sb("m2_sb", [128, 16], F32)
        g1 = sb("g1_sb", [128, 16], F32)
        g2 = sb("g2_sb", [128, 16], F32)
        gdiag = [sb(f"gdiag{i}", [128, 128], F32) for i in range(4)]
        gate_bc = sb("gate_bc_sb", [128, S], F32)
        logT = gate_bc
        st4 = sb("st4_sb", [128, 8, 4], F32)
        vn_t = [sb(f"vn{i}", [128, 512], BF16) for i in range(3)]
        ps = [es.enter_context(nc.psum_tensor(f"ps{i}", [128, TW], F32)) for i in range(8)]

        act = reg[:, 0:8 * S].rearrange("p (f t) -> p f t", f=8)
        CP = S + 32
        cpad = reg[:, 0:4 * CP].rearrange("p (j t) -> p j t", j=4)
        DG0 = 4 * CP
        diag = [reg[:, DG0 + i * CW * 128: DG0 + (i + 1) * CW * 128].rearrange("p (k m) -> p k m", k=CW)
                for i in range(2)]
        UT0 = DG0 + 2 * CW * 128
        u_t = [reg[:, UT0 + i * 2048: UT0 + (i + 1) * 2048].rearrange("p (j t) -> p j t", j=4) for i in range(2)]
        assert UT0 + 2 * 2048 <= REG

        for dry in (True, False):
            sch = Sched(dry=dry)
            B_x = [[Buf(f"x{kc}_{tg}") for tg in range(TG)] for kc in range(KC)]
            B_h = [[Buf(f"h{kc}_{tg}") for tg in range(TG)] for kc in range(KC)]
            B_act = [[Buf() for tg in range(TG)] for f in range(8)]
            B_cp = [[Buf() for tg in range(TG)] for j in range(4)]
            B_cph = Buf("cpad_halo")
            B_diag = [Buf(), Buf()]
            B_ring = [Buf() for _ in range(NSLOT)]
            B_tmp = [Buf() for _ in range(NTMP)]
            B_ps = [Buf() for _ in range(8)]
            B_c = {k: Buf(k) for k in ("vecs", "sgu_gb", "sgu_bb", "sgu_wTf", "sgu_wTb", "consts", "identb",
                                       "ones_m", "ones_1", "blk_m", "router", "rg", "logT", "Ltok", "L2tok",
                                       "eq1", "eq2", "dg", "m1", "m2", "g1", "g2", "gate_bc", "st4")}
            B_c["logT"] = B_c["gate_bc"]
            B_gd = [Buf() for _ in range(4)]
            B_vn = [Buf(), Buf(), Buf()]
            B_u = [Buf(), Buf()]
            B_y = [Buf() for _ in range(16)]
            rr = {"ps": 0, "tmp": 0, "vn": 0, "u": 0, "gd": 0, "dg": 0}

            def nxt(kind, n):
                i = rr[kind]
                rr[kind] = (i + 1) % n
                return i

            rr["ps_s"] = 0
            rr["ps_l"] = 0

            def get_ps(kind=None):
                if kind == "short":
                    i = nxt("ps_s", 5)
                elif kind == "long":
                    i = 5 + nxt("ps_l", 3)
                else:
                    i = nxt("ps", 8)
                return ps[i], B_ps[i]

            def get_tmp():
                i = nxt("tmp", NTMP)
                return tmps[i], B_tmp[i]

            if dry:
                wlist = []
            wstate = {"next_load": 0, "next_use": 0}
            slot_of = {}

            def wload(i):
                src, nf = wlist[i]
                s = i % NSLOT
                dst = ring[s][:, 0:nf, :]
                sch.op("pool", lambda e, dst=dst, src=src: e.dma_start(out=dst, in_=src),
                       writes=[B_ring[s]], dma_key=("ring", s))

            def wget_group(specs):
                if dry:
                    for s_ in specs:
                        wlist.append(s_)
                    return [(ring[0], B_ring[0]) for _ in specs]
                i = wstate["next_use"]
                wstate["next_use"] += len(specs)
                assert len(specs) <= NSLOT
                while wstate["next_load"] < len(wlist) and wstate["next_load"] < i + NSLOT:
                    wload(wstate["next_load"])
                    wstate["next_load"] += 1
                out = []
                for n in range(len(specs)):
                    assert wlist[i + n][1] == specs[n][1]
                    s = (i + n) % NSLOT
                    out.append((ring[s], B_ring[s]))
                    slot_of[id(B_ring[s])] = s
                return out

            def wget(src, nf=8):
                return wget_group([(src, nf)])[0]

            def kview(w2d):
                return w2d.rearrange("(k p) n -> p k n", p=128)

            def fview(w2d):
                return w2d.rearrange("(f p) d -> p f d", p=128)

            def ld(eng, dst, src, buf, key):
                sch.op(eng, lambda e: e.dma_start(out=dst, in_=src), writes=[buf], dma_key=key)

            for tg in range(TG):
                sch.op("sp", lambda e, tg=tg: e.dma_start(out=xT[:, :, tg * TW:(tg + 1) * TW], in_=xT_d[:, :, tg * TW:(tg + 1) * TW]),
                       writes=[B_x[kc][tg] for kc in range(KC)], dma_key=("x", tg))
            ld("act", vecs[:, :], vecs_d[:, :], B_c["vecs"], "vecs")
            ld("act", consts[:, :, :], consts_d[:, :, :], B_c["consts"], "consts")
            ld("act", router[:, :, :], router_d[:, :, :], B_c["router"], "router")

            sch.op("dve", lambda e: e.memset(ones_m[:, :], 1.0 / D), writes=[B_c["ones_m"]])
            sch.op("dve", lambda e: e.memset(ones_1[:, :], 1.0), writes=[B_c["ones_1"]])
            B_c["eps"] = Buf("eps")
            sch.op("dve", lambda e: e.memset(eps_t[:, :], EPS), writes=[B_c["eps"]])

            def rsqrt_eps(out_ap, in_ap, reads, Bout):
                sch.op("act", lambda e: e.activation(out=out_ap, in_=in_ap, func=AF.Ln, bias=eps_t[0:out_ap.shape[0], :]),
                       reads=list(reads) + [B_c["eps"]], writes=[Bout])
                sch.op("act", lambda e: e.activation(out=out_ap, in_=out_ap, func=AF.Exp, scale=-0.5), writes=[Bout])
            sch.op("dve", lambda e: e.memset(blk_m[:, :], 0.0), writes=[B_c["blk_m"]])
            sch.op("dve", lambda e: e.memset(blk_m[0:64, 0:64], 1.0 / 64), writes=[B_c["blk_m"]])
            sch.op("dve", lambda e: e.memset(blk_m[64:128, 64:128], 1.0 / 64), writes=[B_c["blk_m"]])
            sch.op("dve", lambda e: e.tensor_copy(out=identb[:, :], in_=consts[:, 0, :]),
                   reads=[B_c["consts"]], writes=[B_c["identb"]])

            def rmsnorm(gcol, out_bf16=True, final=False, keep_rstd=None, tgs=range(TG)):
                for tg in tgs:
                    tsl = slice(tg * TW, (tg + 1) * TW)
                    pst, Bpst = get_ps()
                    for kc in range(KC):
                        sq, Bsq = get_tmp()
                        sch.op("act", lambda e, sq=sq, kc=kc, tsl=tsl: e.activation(
                            out=sq[:, :], in_=xT[:, kc, tsl], func=AF.Square),
                            reads=[B_x[kc][tg]], writes=[Bsq])
                        sch.op("pe", lambda e, sq=sq, kc=kc, pst=pst: e.matmul(
                            pst[:, :], ones_m[:, :], sq[:, :], start=(kc == 0), stop=(kc == KC - 1)),
                            reads=[Bsq, B_c["ones_m"]], writes=[Bpst])
                    rstd, Brstd = get_tmp()
                    rsqrt_eps(rstd[:, :], pst[:, :], [Bpst], Brstd)
                    if keep_rstd is not None:
                        keep_rstd(tg, rstd, Brstd)
                    for kc in range(KC):
                        if final:
                            sch.op("dve", lambda e, kc=kc, tsl=tsl, rstd=rstd: e.scalar_tensor_tensor(
                                out=xT[:, kc, tsl], in0=xT[:, kc, tsl], scalar=vecs[:, gcol + kc:gcol + kc + 1],
                                in1=rstd[:, :], op0=ALU.mult, op1=ALU.mult),
                                reads=[Brstd, B_c["vecs"]], writes=[B_x[kc][tg]])
                        else:
                            sch.op("dve", lambda e, kc=kc, tsl=tsl, rstd=rstd: e.scalar_tensor_tensor(
                                out=hT[:, kc, tsl], in0=xT[:, kc, tsl], scalar=vecs[:, gcol + kc:gcol + kc + 1],
                                in1=rstd[:, :], op0=ALU.mult, op1=ALU.mult),
                                reads=[Brstd, B_x[kc][tg], B_c["vecs"]], writes=[B_h[kc][tg]])

            def mm_group(pst, Bpst, lhs_fn, rhs_fn, nk, reads, cols=None):
                def f(e):
                    last = None
                    for k in range(nk):
                        o = pst[:, :] if cols is None else pst[:, cols]
                        last = e.matmul(o, lhs_fn(k), rhs_fn(k), start=(k == 0), stop=(k == nk - 1))
                    return last
                sch.op("pe", f, reads=reads, writes=[Bpst])

            pre_normed = {"v": False}
            out_evs = []

            def store_out(tg):
                ev = sch.op("sp", lambda e, tg=tg: e.dma_start(out=yT_d[:, :, tg * TW:(tg + 1) * TW],
                                                               in_=xT[:, :, tg * TW:(tg + 1) * TW]),
                            reads=[B_x[kc][tg] for kc in range(KC)], dma_key=("out", tg))
                out_evs.append(ev)

            for l in range(L):
                ld("act", sgu_gb[:, :, :], sgu_gb_d[:, l, :, :], B_c["sgu_gb"], "sgu_gb")
                ld("act", sgu_bb[:, :, :], sgu_bb_d[:, l, :, :], B_c["sgu_bb"], "sgu_bb")
                ld("act", sgu_wTf[:, :, :], sgu_wT_d[:, l, :, :], B_c["sgu_wTf"], "sgu_wTf")
                def mkw(e):
                    return e.tensor_tensor(out=sgu_wTb[:, :, :], in0=sgu_wTf[:, :, :],
                                           in1=consts[:, 1:2, :].to_broadcast([128, 4, 128]), op=ALU.mult)
                sch.op("dve", mkw, reads=[B_c["sgu_wTf"], B_c["consts"]], writes=[B_c["sgu_wTb"]])

                fence_bufs = [B_cph] + B_diag + B_u + [b for r_ in B_cp for b in r_] + [b for r_ in B_act for b in r_]
                for j in range(4):
                    sch.op("dve", lambda e, j=j: e.memset(cpad[:, j, 0:30], 0.0), writes=fence_bufs if j == 0 else [B_cph])

                if not pre_normed["v"]:
                    rmsnorm(v_nmix(l))
                pre_normed["v"] = False
                if stop_after == ("dbg_h", l):
                    for kc in range(KC):
                        for tg in range(TG):
                            sch.op("dve", lambda e, kc=kc, tg=tg: e.tensor_copy(out=xT[:, kc, tg * TW:(tg + 1) * TW],
                                                                                in_=hT[:, kc, tg * TW:(tg + 1) * TW]),
                                   reads=[B_h[kc][tg]], writes=[B_x[kc][tg]])
                    break
                win = kview(w_in_d[l])

                def c_diag(j):
                    di = j % 2
                    dgm, Bdg = diag[di], B_diag[di]
                    for k in range(CW):
                        c0 = v_cw(l) + j * CW + k
                        sch.op("dve", lambda e, dgm=dgm, k=k, c0=c0: e.tensor_scalar(
                            out=dgm[:, k, :], in0=identb[:, :], scalar1=vecs[:, c0:c0 + 1], scalar2=None, op0=ALU.mult),
                            reads=[B_c["identb"], B_c["vecs"]], writes=[Bdg] if k in (0, CW - 1) else [])

                c_diag(0)
                c_diag(1)

                for j in range(4):
                    (wa, Bwa), (wg_, Bwg) = wget_group([(win[:, :, j * 128:(j + 1) * 128], 8),
                                                        (win[:, :, 512 + j * 128:512 + (j + 1) * 128], 8)])
                    for tg in range(TG):
                        tsl = slice(tg * TW, (tg + 1) * TW)
                        hb = [B_h[kc][tg] for kc in range(KC)]
                        pa, Bpa = get_ps()
                        mm_group(pa, Bpa, lambda k, wa=wa: wa[:, k, :], lambda k, tsl=tsl: hT[:, k, tsl], KC, hb + [Bwa])
                        pg, Bpg = get_ps()
                        mm_group(pg, Bpg, lambda k, wg_=wg_: wg_[:, k, :], lambda k, tsl=tsl: hT[:, k, tsl], KC, hb + [Bwg])
                        sg, Bsg = get_tmp()
                        sch.op("act", lambda e, sg=sg, pg=pg: e.activation(out=sg[:, :], in_=pg[:, :], func=AF.Sigmoid),
                               reads=[Bpg], writes=[Bsg])
                        sch.op("dve", lambda e, sg=sg, pa=pa, j=j, tg=tg: e.tensor_tensor(
                            out=cpad[:, j, 30 + tg * TW:30 + (tg + 1) * TW], in0=pa[:, :], in1=sg[:, :], op=ALU.mult),
                            reads=[Bpa, Bsg, B_cph], writes=[B_cp[j][tg]])

                if stop_after == ("dbg_c", l):
                    for j in range(4):
                        for tg in range(TG):
                            sch.op("dve", lambda e, j=j, tg=tg: e.tensor_copy(out=xT[:, j, tg * TW:(tg + 1) * TW],
                                                                              in_=cpad[:, j, 30 + tg * TW:30 + (tg + 1) * TW]),
                                   reads=[B_cp[j][tg]], writes=[B_x[j][tg]])
                    break
                uv_p = wget_group([(win[:, :, 1024 + j * 128:1024 + (j + 1) * 128], 8) for j in range(8)])
                wu_p, wv_p = uv_p[0:4], uv_p[4:8]
                bstate = {}
                BENG = "pool"

                def b_stage1(tt):
                    tg, t4 = divmod(tt, 4)
                    tsl = slice(tg * TW, (tg + 1) * TW)
                    hb = [B_h[kc][tg] for kc in range(KC)]
                    if t4 == 0:
                        ui = nxt("u", 2)
                        ut, But = u_t[ui], B_u[ui]
                        bstate[("u", tg)] = (ut, But)
                        for j in range(4):
                            pu, Bpu = get_ps()
                            w_, Bw_ = wu_p[j]
                            mm_group(pu, Bpu, lambda k, w_=w_: w_[:, k, :], lambda k, tsl=tsl: hT[:, k, tsl], KC, hb + [Bw_])
                            sch.op("act", lambda e, pu=pu, ut=ut, j=j: e.activation(out=ut[:, j, :], in_=pu[:, :], func=AF.Gelu),
                                   reads=[Bpu], writes=[But])
                    ksl = slice(tt * 128, (tt + 1) * 128)
                    pv, Bpv = get_ps()
                    vs0 = 0 if dry else slot_of[id(wv_p[0][1])]
                    if not dry:
                        assert [slot_of[id(wv_p[hd][1])] for hd in range(4)] == [vs0 + hd for hd in range(4)]
                    def fv(e, pv=pv, ksl=ksl, vs0=vs0):
                        last = None
                        for k in range(KC):
                            last = e.matmul(pv[:, :], hT[:, k, ksl], ring_all[:, vs0:vs0 + 4, k, :],
                                            start=(k == 0), stop=(k == KC - 1))
                        return last
                    sch.op("pe", fv, reads=hb + [wv_p[hd][1] for hd in range(4)], writes=[Bpv])
                    gv, Bgv = get_tmp()
                    sch.op("act", lambda e, gv=gv, pv=pv: e.activation(out=gv[:, :], in_=pv[:, :], func=AF.Gelu),
                           reads=[Bpv], writes=[Bgv])
                    sq, Bsq = get_tmp()
                    sch.op("dve", lambda e, sq=sq, gv=gv: e.tensor_tensor(out=sq[:, :], in0=gv[:, :], in1=gv[:, :], op=ALU.mult),
                           reads=[Bgv], writes=[Bsq])
                    si = tt % 8
                    Bm, Bs4 = B_c["m1"], B_c["st4"]
                    gv3 = gv[:, :].rearrange("p (h c) -> p h c", h=4)
                    sq3 = sq[:, :].rearrange("p (h c) -> p h c", h=4)
                    sch.op("dve", lambda e, gv3=gv3: e.tensor_reduce(out=m1[:, 0:4], in_=gv3, axis=AX.X, op=ALU.add),
                           reads=[Bgv], writes=[Bm])
                    sch.op("dve", lambda e, sq3=sq3: e.tensor_reduce(out=m1[:, 4:8], in_=sq3, axis=AX.X, op=ALU.add),
                           reads=[Bsq], writes=[Bm])
                    sch.op(BENG, lambda e: e.tensor_scalar(out=m1[:, 0:8], in0=m1[:, 0:8], scalar1=1.0 / 128, scalar2=None, op0=ALU.mult),
                           writes=[Bm])
                    sch.op(BENG, lambda e: e.tensor_tensor(out=m1[:, 8:12], in0=m1[:, 0:4], in1=m1[:, 0:4], op=ALU.mult),
                           writes=[Bm])
                    sch.op(BENG, lambda e: e.tensor_tensor(out=m1[:, 4:8], in0=m1[:, 4:8], in1=m1[:, 8:12], op=ALU.subtract),
                           writes=[Bm])
                    rsqrt_eps(st4[:, si, :], m1[:, 4:8], [Bm], Bs4)
                    for hd in range(4):
                        sch.op("dve", lambda e, hd=hd, sq=sq, gv=gv, si=si: e.tensor_scalar(
                            out=sq[:, hd * 128:(hd + 1) * 128], in0=gv[:, hd * 128:(hd + 1) * 128],
                            scalar1=m1[:, hd:hd + 1], scalar2=st4[:, si, hd:hd + 1], op0=ALU.subtract, op1=ALU.mult),
                            reads=[Bgv, Bm, Bs4], writes=[Bsq])
                    vi = nxt("vn", 3)
                    vn, Bvn = vn_t[vi], B_vn[vi]
                    sch.op(BENG, lambda e, sq=sq: e.tensor_tensor(out=sq[:, :], in0=sq[:, :], in1=sgu_gb[:, 0, :], op=ALU.mult),
                           reads=[B_c["sgu_gb"]], writes=[Bsq])
                    sch.op(BENG, lambda e, sq=sq, vn=vn: e.tensor_tensor(out=vn[:, :], in0=sq[:, :], in1=sgu_gb[:, 1, :], op=ALU.add),
                           reads=[Bsq, B_c["sgu_gb"]], writes=[Bvn])
                    bstate[("vn", tt)] = (vn, Bvn)

                def b_stage2(tt):
                    tg, t4 = divmod(tt, 4)
                    ksl = slice(tt * 128, (tt + 1) * 128)
                    ut, But = bstate[("u", tg)]
                    vn, Bvn = bstate[("vn", tt)]
                    pp, Bpp = get_ps()
                    def fsp(e, pp=pp, vn=vn):
                        last = None
                        for hd in range(4):
                            last = e.matmul(pp[:, hd * 128:(hd + 1) * 128], vn[:, hd * 128:(hd + 1) * 128],
                                            sgu_wTb[:, hd, :], start=True, stop=True)
                        return last
                    sch.op("pe", fsp, reads=[Bvn, B_c["sgu_wTb"]], writes=[Bpp])
                    tb, Btb = get_tmp()
                    sch.op("dve", lambda e, pp=pp, tb=tb: e.tensor_tensor(
                        out=tb[:, :], in0=pp[:, :], in1=sgu_bb[:, :, :].rearrange("p h t -> p (h t)"), op=ALU.add),
                        reads=[Bpp, B_c["sgu_bb"]], writes=[Btb])
                    sch.op("dve", lambda e, tb=tb, ut=ut, t4=t4, ksl=ksl: e.tensor_tensor(
                        out=hT[:, 4:8, ksl], in0=tb[:, :].rearrange("p (h t) -> p h t", h=4),
                        in1=ut[:, :, t4 * 128:(t4 + 1) * 128], op=ALU.mult),
                        reads=[Btb, But], writes=[B_y[tt]], war=[B_h[kc][tg] for kc in range(4, 8)])


                cstate = {}

                def c_stage1(j, tg):
                    di = j % 2
                    dgm, Bdg = diag[di], B_diag[di]
                    pc, Bpc = get_ps()
                    rd = [Bdg, B_cp[j][tg], B_cph] + ([B_cp[j][tg - 1]] if tg > 0 else [])
                    mm_group(pc, Bpc, lambda k, dgm=dgm: dgm[:, k, :],
                             lambda k, j=j, tg=tg: cpad[:, j, tg * TW + k: tg * TW + k + TW], CW, rd)
                    y, By = get_tmp()
                    ysq, Bysq = get_tmp()
                    cb = vecs[:, v_cb(l) + j: v_cb(l) + j + 1]
                    sch.op("act", lambda e, y=y, pc=pc, cb=cb: e.activation(out=y[:, :], in_=pc[:, :], func=AF.Identity, bias=cb),
                           reads=[Bpc, B_c["vecs"]], writes=[By])
                    sch.op("act", lambda e, ysq=ysq, y=y: e.activation(out=ysq[:, :], in_=y[:, :], func=AF.Square),
                           reads=[By], writes=[Bysq])
                    cstate[(j, tg)] = (y, By, ysq, Bysq)

                def c_stage2(j, tg):
                    y, By, ysq, Bysq = cstate[(j, tg)]
                    pm1, Bpm1 = get_ps()
                    pm2, Bpm2 = get_ps()
                    sch.op("pe", lambda e, pm1=pm1, y=y: e.matmul(pm1[:, :], blk_m[:, :], y[:, :], start=True, stop=True),
                           reads=[By, B_c["blk_m"]], writes=[Bpm1])
                    sch.op("pe", lambda e, pm2=pm2, ysq=ysq: e.matmul(pm2[:, :], blk_m[:, :], ysq[:, :], start=True, stop=True),
                           reads=[Bysq, B_c["blk_m"]], writes=[Bpm2])
                    sch.op("act", lambda e, ysq=ysq, pm1=pm1: e.activation(out=ysq[:, :], in_=pm1[:, :], func=AF.Square),
                           reads=[Bpm1], writes=[Bysq])
                    sch.op("dve", lambda e, ysq=ysq, pm2=pm2: e.tensor_tensor(out=ysq[:, :], in0=pm2[:, :], in1=ysq[:, :], op=ALU.subtract),
                           reads=[Bpm2], writes=[Bysq])
                    sch.op("dve", lambda e, y=y, pm1=pm1: e.tensor_tensor(out=y[:, :], in0=y[:, :], in1=pm1[:, :], op=ALU.subtract),
                           reads=[Bpm1], writes=[By])
                    rsqrt_eps(ysq[:, :], ysq[:, :], [], Bysq)
                    sch.op("dve", lambda e, y=y, ysq=ysq: e.tensor_tensor(out=y[:, :], in0=y[:, :], in1=ysq[:, :], op=ALU.mult),
                           reads=[Bysq], writes=[By])
                    ng = vecs[:, v_cng(l) + j: v_cng(l) + j + 1]
                    nb = vecs[:, v_cnb(l) + j: v_cnb(l) + j + 1]
                    sch.op("act", lambda e, y=y, j=j, tg=tg, ng=ng, nb=nb: e.activation(
                        out=cpad[:, j, 30 + tg * TW:30 + (tg + 1) * TW], in_=y[:, :], func=AF.Silu, bias=nb, scale=ng),
                        reads=[By, B_c["vecs"]], writes=[B_cp[j][tg]])

                ctiles = [(j, tg) for j in range(4) for tg in range(TG - 1, -1, -1)]
                b_seq = []
                BD = 2
                for tt in range(16):
                    step = [lambda tt=tt: b_stage1(tt)]
                    if tt >= BD:
                        step.append(lambda tt=tt: b_stage2(tt - BD))
                    b_seq.append(step)
                for tt in range(16 - BD, 16):
                    b_seq.append([lambda tt=tt: b_stage2(tt)])
                c_seq = [[]]
                for i, (j, tg) in enumerate(ctiles):
                    step = [lambda j=j, tg=tg: c_stage1(j, tg)]
                    if i >= 1:
                        pj, ptg = ctiles[i - 1]
                        step.append(lambda pj=pj, ptg=ptg: c_stage2(pj, ptg))
                        if ptg == 0 and pj + 2 < 4:
                            step.append(lambda pj=pj: c_diag(pj + 2))
                    c_seq.append(step)
                c_seq.append([lambda: c_stage2(*ctiles[-1])])
                for i in range(max(len(b_seq), len(c_seq))):
                    if i < len(b_seq):
                        for f_ in b_seq[i]:
                            f_()
                    if i < len(c_seq):
                        for f_ in c_seq[i]:
                            f_()

                wout = kview(w_out_d[l])

                def phase_d(tgs):
                    for dc in range(KC):
                        wo, Bwo = wget(wout[:, :, dc * 128:(dc + 1) * 128])
                        for tg in tgs:
                            tsl = slice(tg * TW, (tg + 1) * TW)
                            po, Bpo = get_ps()
                            mm_group(po, Bpo, lambda k, wo=wo: wo[:, k, :],
                                     lambda k, tsl=tsl, tg=tg: (cpad[:, k, 30 + tg * TW:30 + (tg + 1) * TW] if k < 4 else hT[:, k, tsl]), KC,
                                     [B_cp[k][tg] for k in range(4)] + [B_h[kc][tg] for kc in range(4, KC)]
                                     + [B_y[tg * 4 + q] for q in range(4)] + [Bwo])
                            sch.op("dve", lambda e, po=po, dc=dc, tsl=tsl: e.tensor_tensor(
                                out=xT[:, dc, tsl], in0=xT[:, dc, tsl], in1=po[:, :], op=ALU.add),
                                reads=[Bpo], writes=[B_x[dc][tg]])

                if stop_after == ("mixer", l):
                    phase_d(range(TG))
                    break

                moe = (l % 2 == 1)
                keep_fn = None
                if moe:
                    for kc in range(KC):
                        c0 = v_nffn(l) + kc
                        sch.op("dve", lambda e, kc=kc, c0=c0: e.tensor_scalar(
                            out=rg[:, kc, :], in0=router[:, kc, :], scalar1=vecs[:, c0:c0 + 1], scalar2=None, op0=ALU.mult),
                            reads=[B_c["router"], B_c["vecs"]], writes=[B_c["rg"]])
                    def keep8(tg, rstd, Brstd):
                        tsl = slice(tg * TW, (tg + 1) * TW)
                        pl, Bpl = get_ps()
                        def f(e, pl=pl, tsl=tsl):
                            last = None
                            for k in range(KC):
                                last = e.matmul(pl[0:8, :], rg[:, k, :], xT[:, k, tsl], start=(k == 0), stop=(k == KC - 1))
                            return last
                        sch.op("pe", f, reads=[B_x[kc][tg] for kc in range(KC)] + [B_c["rg"]], writes=[Bpl])
                        sch.op("dve", lambda e, pl=pl, rstd=rstd, tsl=tsl: e.tensor_tensor(
                            out=logT[0:8, tsl], in0=pl[0:8, :], in1=rstd[0:8, :], op=ALU.mult),
                            reads=[Bpl, Brstd], writes=[B_c["logT"]])
                    keep_fn = keep8
                for tgs_ in TSPLIT:
                    phase_d(tgs_)
                    rmsnorm(v_nffn(l), keep_rstd=keep_fn, tgs=tgs_)
                if moe:
                    pt, Bpt = get_ps()
                    def ftr(e, pt=pt):
                        last = None
                        for tt in range(16):
                            last = e.matmul(pt[:, tt * 8:(tt + 1) * 8], logT[0:8, tt * 128:(tt + 1) * 128],
                                            consts[0:8, 0, 0:8], start=True, stop=True)
                        return last
                    sch.op("pe", ftr, reads=[B_c["logT"], B_c["consts"]], writes=[Bpt])
                    Bc = B_c
                    Lv = Ltok[:, :, :]
                    bc3 = lambda t: t[:, :].unsqueeze(2).to_broadcast([128, 16, NE])
                    sch.op("dve", lambda e, pt=pt: e.tensor_copy(out=Lv, in_=pt[:, 0:128].rearrange("p (t e) -> p t e", e=NE)),
                           reads=[Bpt], writes=[Bc["Ltok"]])
                    sch.op("dve", lambda e: e.tensor_reduce(out=m1[:, :], in_=Lv, axis=AX.X, op=ALU.max),
                           reads=[Bc["Ltok"]], writes=[Bc["m1"]])
                    sch.op("dve", lambda e: e.tensor_tensor(out=eq1[:, :, :], in0=Lv, in1=bc3(m1), op=ALU.is_equal),
                           reads=[Bc["Ltok"], Bc["m1"]], writes=[Bc["eq1"]])
                    sch.op("dve", lambda e: e.scalar_tensor_tensor(out=L2tok[:, :, :], in0=eq1[:, :, :], scalar=-1e30, in1=Lv,
                                                                   op0=ALU.mult, op1=ALU.add),
                           reads=[Bc["Ltok"], Bc["eq1"]], writes=[Bc["L2tok"]])
                    sch.op("dve", lambda e: e.tensor_reduce(out=m2[:, :], in_=L2tok[:, :, :], axis=AX.X, op=ALU.max),
                           reads=[Bc["L2tok"]], writes=[Bc["m2"]])
                    sch.op("dve", lambda e: e.tensor_tensor(out=eq2[:, :, :], in0=L2tok[:, :, :], in1=bc3(m2), op=ALU.is_equal),
                           reads=[Bc["L2tok"], Bc["m2"]], writes=[Bc["eq2"]])
                    sch.op("dve", lambda e: e.tensor_tensor(out=g2[:, :], in0=m1[:, :], in1=m2[:, :], op=ALU.subtract),
                           reads=[Bc["m1"], Bc["m2"]], writes=[Bc["g2"]])
                    sch.op("act", lambda e: e.activation(out=g1[:, :], in_=g2[:, :], func=AF.Sigmoid),
                           reads=[Bc["g2"]], writes=[Bc["g1"]])
                    sch.op("act", lambda e: e.activation(out=m2[:, :], in_=g2[:, :], func=AF.Sigmoid, scale=-1.0),
                           reads=[Bc["g2"]], writes=[Bc["m2"]])
                    sch.op("dve", lambda e: e.tensor_tensor(out=eq1[:, :, :], in0=eq1[:, :, :], in1=bc3(g1), op=ALU.mult),
                           reads=[Bc["g1"]], writes=[Bc["eq1"]])
                    sch.op("dve", lambda e: e.tensor_tensor(out=eq2[:, :, :], in0=eq2[:, :, :], in1=bc3(m2), op=ALU.mult),
                           reads=[Bc["m2"]], writes=[Bc["eq2"]])
                    sch.op("dve", lambda e: e.tensor_tensor(out=dg[:, :, :], in0=eq1[:, :, :], in1=eq2[:, :, :], op=ALU.add),
                           reads=[Bc["eq1"], Bc["eq2"]], writes=[Bc["dg"]])

                if stop_after == ("norm2", l):
                    break

                sch.op("dve", lambda e: e.memset(st4[:, 0, :], 0.0),
                       writes=[B_c["st4"], B_cph] + B_diag + B_u + [b for r_ in B_cp for b in r_] + [b for r_ in B_act for b in r_])
                n_exp = NE if moe else 1
                for ex in range(n_exp):
                    if moe:
                        wg_d, wu_d, wd_d = kview(moe_wg_d[0, ex]), kview(moe_wu_d[0, ex]), fview(moe_wd_d[0, ex])
                    else:
                        wg_d, wu_d, wd_d = kview(ffn_wg_d[0]), kview(ffn_wu_d[0]), fview(ffn_wd_d[0])
                    gate_ready = False
                    for (f0, nf) in FBLOCKS:
                        for fl in range(nf):
                            fc = f0 + fl
                            (wg_, Bwg), (wu_, Bwu) = wget_group([(wg_d[:, :, fc * 128:(fc + 1) * 128], 8),
                                                                 (wu_d[:, :, fc * 128:(fc + 1) * 128], 8)])
                            for tg in range(TG):
                                tsl = slice(tg * TW, (tg + 1) * TW)
                                hb = [B_h[kc][tg] for kc in range(KC)]
                                pg, Bpg = get_ps()
                                mm_group(pg, Bpg, lambda k, wg_=wg_: wg_[:, k, :], lambda k, tsl=tsl: hT[:, k, tsl], KC, hb + [Bwg])
                                pu, Bpu = get_ps()
                                mm_group(pu, Bpu, lambda k, wu_=wu_: wu_[:, k, :], lambda k, tsl=tsl: hT[:, k, tsl], KC, hb + [Bwu])
                                sg, Bsg = get_tmp()
                                sch.op("act", lambda e, sg=sg, pg=pg: e.activation(out=sg[:, :], in_=pg[:, :], func=AF.Silu),
                                       reads=[Bpg], writes=[Bsg])
                                sch.op("dve", lambda e, sg=sg, pu=pu, fl=fl, tsl=tsl: e.tensor_tensor(
                                    out=act[:, fl, tsl], in0=pu[:, :], in1=sg[:, :], op=ALU.mult),
                                    reads=[Bpu, Bsg], writes=[B_act[fl][tg]])
                            if moe and not gate_ready and fl == 1:
                                gate_ready = True
                                for tg in range(TG):
                                    pgt, Bpgt = get_ps()
                                    for t4 in range(4):
                                        tt = tg * 4 + t4
                                        gi = nxt("gd", 4)
                                        sch.op("dve", lambda e, gi=gi, tt=tt, ex=ex: e.tensor_scalar(
                                            out=gdiag[gi][:, :], in0=consts[:, 0, :], scalar1=dg[:, tt, ex:ex + 1],
                                            scalar2=None, op0=ALU.mult),
                                            reads=[B_c["dg"], B_c["consts"]], writes=[B_gd[gi]])
                                        sch.op("pe", lambda e, gi=gi, pgt=pgt, t4=t4: e.matmul(
                                            pgt[:, t4 * 128:(t4 + 1) * 128], ones_1[:, :], gdiag[gi][:, :], start=True, stop=True),
                                            reads=[B_gd[gi], B_c["ones_1"]], writes=[Bpgt])
                                    sch.op("act", lambda e, pgt=pgt, tg=tg: e.activation(
                                        out=gate_bc[:, tg * TW:(tg + 1) * TW], in_=pgt[:, :], func=AF.Identity),
                                        reads=[Bpgt], writes=[B_c["gate_bc"]])
                        def down_proj(tgs, f0=f0, nf=nf, wd_d=wd_d):
                            for dc in range(KC):
                                wd_, Bwd = wget(wd_d[:, f0:f0 + nf, dc * 128:(dc + 1) * 128], nf)
                                for tg in tgs:
                                    tsl = slice(tg * TW, (tg + 1) * TW)
                                    po, Bpo = get_ps()
                                    mm_group(po, Bpo, lambda k, wd_=wd_: wd_[:, k, :], lambda k, tsl=tsl: act[:, k, tsl], nf,
                                             [B_act[k][tg] for k in range(nf)] + [Bwd])
                                    if moe:
                                        tb, Btb = get_tmp()
                                        sch.op("dve", lambda e, tb=tb, po=po, tsl=tsl: e.tensor_tensor(
                                            out=tb[:, :], in0=po[:, :], in1=gate_bc[:, tsl], op=ALU.mult),
                                            reads=[Bpo, B_c["gate_bc"]], writes=[Btb])
                                        sch.op("dve", lambda e, tb=tb, dc=dc, tsl=tsl: e.tensor_tensor(
                                            out=xT[:, dc, tsl], in0=xT[:, dc, tsl], in1=tb[:, :], op=ALU.add),
                                            reads=[Btb], writes=[B_x[dc][tg]])
                                    else:
                                        sch.op("dve", lambda e, po=po, dc=dc, tsl=tsl: e.tensor_tensor(
                                            out=xT[:, dc, tsl], in0=xT[:, dc, tsl], in1=po[:, :], op=ALU.add),
                                            reads=[Bpo], writes=[B_x[dc][tg]])

                        last_block = (ex == n_exp - 1 and f0 == FBLOCKS[-1][0])
                        if last_block and stop_after is None:
                            for tgs_ in TSPLIT:
                                down_proj(tgs_)
                                if l + 1 < L:
                                    rmsnorm(v_nmix(l + 1), tgs=tgs_)
                                else:
                                    rmsnorm(V_NFIN, final=True, tgs=tgs_)
                                    for tg in tgs_:
                                        store_out(tg)
                            pre_normed["v"] = True
                        else:
                            down_proj(range(TG))
                    if stop_after == ("expert", l, ex):
                        break
                if stop_after is not None and stop_after[0] in ("ffn", "expert") and stop_after[1] == l:
                    break

            if stop_after is None:
                if not pre_normed["v"]:
                    rmsnorm(V_NFIN, final=True)
                    for tg in range(TG):
                        store_out(tg)
            else:
                for tg in range(TG):
                    store_out(tg)
            if not dry:
                for ev in out_evs:
                    b = Buf()
                    b.w = ev
                    sch.op("sp", lambda e: e.nop(), reads=[b])

        build_program.last_counts = dict(sch.cnt)
        sem = {}
        for e in Sched.ENGS:
            sem[e] = es.enter_context(nc.semaphore(f"s_{e}"))
        for n_, key in enumerate(sch.dma_cnt):
            sem[key] = es.enter_context(nc.semaphore(f"d_{n_}"))
        block = es.enter_context(nc.Block())

        def run(handle, items):
            for waits, fn, ev in items:
                for k, v in waits:
                    handle.wait_ge(sem[k], v)
                inst = fn(handle)
                if ev is not None:
                    inst.then_inc(sem[ev[0]], 16 if isinstance(ev[0], tuple) else 1)

        @block.tensor
        def _(t):
            run(t, sch.q["pe"])

        @block.scalar
        def _(a):
            run(a, sch.q["act"])

        @block.vector
        def _(v):
            run(v, sch.q["dve"])

        @block.gpsimd
        def _(g):
            run(g, sch.q["pool"])

        @block.sync
        def _(s):
            run(s, sch.q["sp"])
    return nc


def prep_inputs(inp):
    f = lambda a: np.ascontiguousarray(np.asarray(a, dtype=np.float32))
    x = f(inp["x"])
    B = x.shape[0]
    xT = [np.ascontiguousarray(x[b].T.reshape(KC, 128, S).transpose(1, 0, 2)) for b in range(B)]
    vecs = np.zeros((128, NV), np.float32)
    def cols8(v):
        return np.asarray(v, np.float32).reshape(8, 128).T
    def cols4(v):
        return np.asarray(v, np.float32).reshape(4, 128).T
    for l in range(L):
        vecs[:, v_nmix(l):v_nmix(l) + 8] = cols8(inp["norm_mix"][l])
        vecs[:, v_nffn(l):v_nffn(l) + 8] = cols8(inp["norm_ffn"][l])
        vecs[:, v_cb(l):v_cb(l) + 4] = cols4(inp["conv_b"][l])
        vecs[:, v_cng(l):v_cng(l) + 4] = cols4(inp["conv_ng"][l])
        vecs[:, v_cnb(l):v_cnb(l) + 4] = cols4(inp["conv_nb"][l])
        cw = np.asarray(inp["conv_w"][l], np.float32).reshape(CW, 4, 128)
        vecs[:, v_cw(l):v_cw(l) + 4 * CW] = cw.transpose(2, 1, 0).reshape(128, 4 * CW)
    vecs[:, V_NFIN:V_NFIN + 8] = cols8(inp["norm_final"])
    sgu_gb = np.empty((128, L, 2, 512), np.float32)
    sgu_gb[:, :, 0, :] = np.asarray(inp["sgu_ng"], np.float32)[None]
    sgu_gb[:, :, 1, :] = np.asarray(inp["sgu_nb"], np.float32)[None]
    sgu_bb = np.ascontiguousarray(np.broadcast_to(np.asarray(inp["sgu_b"], np.float32)[None], (128, L, 4, 128)))
    sgu_wT = np.ascontiguousarray(np.asarray(inp["sgu_w"], np.float32).transpose(3, 0, 1, 2))
    consts = np.zeros((128, 2, 128), np.float32)
    consts[:, 0, :] = np.eye(128, dtype=np.float32)
    consts[:, 1, :] = np.triu(np.ones((128, 128), np.float32))
    router = np.ascontiguousarray(np.asarray(inp["moe_router"], np.float32)[0].reshape(KC, 128, NE).transpose(1, 0, 2))
    shared = dict(vecs=vecs, sgu_gb=sgu_gb, sgu_bb=sgu_bb, sgu_wT=sgu_wT, consts=consts, router=router,
                  w_in=f(inp["w_in"]), w_out=f(inp["w_out"]), ffn_wg=f(inp["ffn_wg"]), ffn_wu=f(inp["ffn_wu"]),
                  ffn_wd=f(inp["ffn_wd"]), moe_wg=f(inp["moe_wg"]), moe_wu=f(inp["moe_wu"]), moe_wd=f(inp["moe_wd"]))
    return xT, shared


def unpack_out(yT):
    return np.ascontiguousarray(yT.transpose(1, 0, 2).reshape(D, S).T)


_NC_CACHE = {}


def kernel(**inputs):
    xT, shared = prep_inputs(inputs)
    B = len(xT)
    if "nc" not in _NC_CACHE:
        _NC_CACHE["nc"] = build_program()
    nc = _NC_CACHE["nc"]
    in_maps = [dict(shared, xT=xT[b]) for b in range(B)]
    res = run_bass_kernel_spmd(nc, in_maps, core_ids=list(range(B)))
    out = np.stack([unpack_out(res.results[b]["yT"]) for b in range(B)], axis=0)
    return out.astype(np.float32)
```

```python
import numpy as np
from contextlib import ExitStack

import concourse.bass as bass
import concourse.mybir as mybir
from concourse.bass_utils import run_bass_kernel_spmd

F32 = mybir.dt.float32
BF16 = mybir.dt.bfloat16
AF = mybir.ActivationFunctionType
ALU = mybir.AluOpType
AX = mybir.AxisListType

D = 1024
S = 2048
L = 2
KC = 8
TG = 4
TW = 512
DFF = 2816
NFC = 22
NE = 8
CW = 31
EPS = 1e-6
FBLOCKS = [(0, 8), (8, 8), (16, 6)]
NSLOT = 8
NTMP = 12
TSPLIT = ([0], [1], [2], [3])

V_PER_L = 8 + 8 + 4 + 4 + 4 + 4 * CW
def v_nmix(l): return l * V_PER_L
def v_nffn(l): return l * V_PER_L + 8
def v_cb(l): return l * V_PER_L + 16
def v_cng(l): return l * V_PER_L + 20
def v_cnb(l): return l * V_PER_L + 24
def v_cw(l): return l * V_PER_L + 28
V_NFIN = L * V_PER_L
NV = V_NFIN + 8

SAME_ENGINE_SYNC = True


class Buf:
    __slots__ = ("w", "r", "name")

    def __init__(self, name=""):
        self.w = None
        self.r = []
        self.name = name


class Sched:
    ENGS = ("pe", "act", "dve", "pool", "sp")

    def __init__(self, dry=False):
        self.dry = dry
        self.q = {e: [] for e in self.ENGS}
        self.cnt = {e: 0 for e in self.ENGS}
        self.seen = {e: {} for e in self.ENGS}
        self.dma_cnt = {}

    def op(self, eng, fn, reads=(), writes=(), dma_key=None, war=()):
        if self.dry:
            return None
        deps = {}
        def add(ev):
            if ev is None:
                return
            k, v = ev
            if deps.get(k, 0) < v:
                deps[k] = v
        for b in reads:
            add(b.w)
        for b in list(writes) + list(war):
            add(b.w)
            for ev in b.r:
                add(ev)
        waits = []
        for k, v in deps.items():
            if k == eng and dma_key is None and not SAME_ENGINE_SYNC:
                continue
            if self.seen[eng].get(k, 0) >= v:
                continue
            self.seen[eng][k] = v
            waits.append((k, v))
        if dma_key is not None:
            key = ("dma", dma_key)
            self.dma_cnt[key] = self.dma_cnt.get(key, 0) + 16
            ev = (key, self.dma_cnt[key])
        else:
            self.cnt[eng] += 1
            ev = (eng, self.cnt[eng])
        self.q[eng].append((waits, fn, ev))
        for b in reads:
            b.r.append(ev)
        for b in writes:
            b.w = ev
            b.r = []
        return ev


def build_program(stop_after=None):
    nc = bass.Bass("TRN2", target_bir_lowering=False)
    dram = {}
    def din(name, shape):
        dram[name] = nc.dram_tensor(name, list(shape), F32, kind="ExternalInput").ap()
        return dram[name]
    xT_d = din("xT", [128, KC, S])
    vecs_d = din("vecs", [128, NV])
    sgu_gb_d = din("sgu_gb", [128, L, 2, 512])
    sgu_bb_d = din("sgu_bb", [128, L, 4, 128])
    sgu_wT_d = din("sgu_wT", [128, L, 4, 128])
    consts_d = din("consts", [128, 2, 128])
    router_d = din("router", [128, KC, NE])
    w_in_d = din("w_in", [L, D, 2 * D])
    w_out_d = din("w_out", [L, D, D])
    ffn_wg_d = din("ffn_wg", [1, D, DFF])
    ffn_wu_d = din("ffn_wu", [1, D, DFF])
    ffn_wd_d = din("ffn_wd", [1, DFF, D])
    moe_wg_d = din("moe_wg", [1, NE, D, DFF])
    moe_wu_d = din("moe_wu", [1, NE, D, DFF])
    moe_wd_d = din("moe_wd", [1, NE, DFF, D])
    yT_d = nc.dram_tensor("yT", [128, KC, S], F32, kind="ExternalOutput").ap()

    es = ExitStack()
    with es:
        def sb(name, shape, dt):
            return es.enter_context(nc.sbuf_tensor(name, list(shape), dt))
        xT = sb("xT_sb", [128, KC, S], F32)
        hT = sb("hT_sb", [128, KC, S], BF16)
        REG = 8 * S + 4096
        reg = sb("reg_sb", [128, REG], BF16)
        ring_all = sb("ring", [128, NSLOT, 8, 128], BF16)
        ring = [ring_all[:, i, :, :] for i in range(NSLOT)]
        tmps = [sb(f"tmp{i}", [128, TW], F32) for i in range(NTMP)]
        vecs = sb("vecs_sb", [128, NV], F32)
        sgu_gb = sb("sgu_gb_sb", [128, 2, 512], F32)
        sgu_bb = sb("sgu_bb_sb", [128, 4, 128], F32)
        sgu_wTf = sb("sgu_wTf_sb", [128, 4, 128], F32)
        sgu_wTb = sb("sgu_wTb_sb", [128, 4, 128], BF16)
        consts = sb("consts_sb", [128, 2, 128], F32)
        identb = sb("identb_sb", [128, 128], BF16)
        ones_m = sb("ones_m_sb", [128, 128], F32)
        ones_1 = sb("ones_1_sb", [128, 128], F32)
        blk_m = sb("blk_m_sb", [128, 128], F32)
        router = sb("router_sb", [128, KC, NE], F32)
        eps_t = sb("eps_sb", [128, 1], F32)
        rg = sb("rg_sb", [128, KC, NE], F32)
        Ltok = sb("Ltok_sb", [128, 16, NE], F32)
        L2tok = sb("L2tok_sb", [128, 16, NE], F32)
        eq1 = sb("eq1_sb", [128, 16, NE], F32)
        eq2 = sb("eq2_sb", [128, 16, NE], F32)
        dg = sb("dg_sb", [128, 16, NE], F32)
        m1 = sb("m1_sb", [128, 16], F32)
        m2 = sb("m2_sb", [128, 16], F32)
        g1 = sb("g1_sb", [128, 16], F32)
        g2 = sb("g2_sb", [128, 16], F32)
        gdiag = [sb(f"gdiag{i}", [128, 128], F32) for i in range(4)]
        gate_bc = sb("gate_bc_sb", [128, S], F32)
        logT = gate_bc
        st4 = sb("st4_sb", [128, 8, 4], F32)
        vn_t = [sb(f"vn{i}", [128, 512], BF16) for i in range(4)]
        ps = [es.enter_context(nc.psum_tensor(f"ps{i}", [128, TW], F32)) for i in range(8)]

        act = reg[:, 0:8 * S].rearrange("p (f t) -> p f t", f=8)
        CP = S + 32
        cpad = reg[:, 0:4 * CP].rearrange("p (j t) -> p j t", j=4)
        DG0 = 4 * CP
        diag = [reg[:, DG0 + i * CW * 128: DG0 + (i + 1) * CW * 128].rearrange("p (k m) -> p k m", k=CW)
                for i in range(2)]
        UT0 = DG0 + 2 * CW * 128
        u_t = [reg[:, UT0 + i * 2048: UT0 + (i + 1) * 2048].rearrange("p (j t) -> p j t", j=4) for i in range(2)]
        assert UT0 + 2 * 2048 <= REG

        for dry in (True, False):
            sch = Sched(dry=dry)
            B_x = [[Buf(f"x{kc}_{tg}") for tg in range(TG)] for kc in range(KC)]
            B_h = [[Buf(f"h{kc}_{tg}") for tg in range(TG)] for kc in range(KC)]
            B_act = [[Buf() for tg in range(TG)] for f in range(8)]
            B_cp = [[Buf() for tg in range(TG)] for j in range(4)]
            B_cph = Buf("cpad_halo")
            B_diag = [Buf(), Buf()]
            B_ring = [Buf() for _ in range(NSLOT)]
            B_tmp = [Buf() for _ in range(NTMP)]
            B_ps = [Buf() for _ in range(8)]
            B_c = {k: Buf(k) for k in ("vecs", "sgu_gb", "sgu_bb", "sgu_wTf", "sgu_wTb", "consts", "identb",
                                       "ones_m", "ones_1", "blk_m", "router", "rg", "logT", "Ltok", "L2tok",
                                       "eq1", "eq2", "dg", "m1", "m2", "g1", "g2", "gate_bc", "st4")}
            B_c["logT"] = B_c["gate_bc"]
            B_gd = [Buf() for _ in range(4)]
            B_vn = [Buf(), Buf(), Buf(), Buf()]
            B_u = [Buf(), Buf()]
            B_y = [Buf() for _ in range(16)]
            rr = {"ps": 0, "tmp": 0, "vn": 0, "u": 0, "gd": 0, "dg": 0}

            def nxt(kind, n):
                i = rr[kind]
                rr[kind] = (i + 1) % n
                return i

            rr["ps_s"] = 0
            rr["ps_l"] = 0

            def get_ps(kind=None):
                if kind == "short":
                    i = nxt("ps_s", 5)
                elif kind == "long":
                    i = 5 + nxt("ps_l", 3)
                else:
                    i = nxt("ps", 8)
                return ps[i], B_ps[i]

            def get_tmp():
                i = nxt("tmp", NTMP)
                return tmps[i], B_tmp[i]

            if dry:
                wlist = []
            wstate = {"next_load": 0, "next_use": 0}
            slot_of = {}

            def wload(i):
                src, nf = wlist[i]
                s = i % NSLOT
                dst = ring[s][:, 0:nf, :]
                sch.op("pool", lambda e, dst=dst, src=src: e.dma_start(out=dst, in_=src),
                       writes=[B_ring[s]], dma_key=("ring", s))

            def wget_group(specs):
                if dry:
                    for s_ in specs:
                        wlist.append(s_)
                    return [(ring[0], B_ring[0]) for _ in specs]
                i = wstate["next_use"]
                wstate["next_use"] += len(specs)
                assert len(specs) <= NSLOT
                while wstate["next_load"] < len(wlist) and wstate["next_load"] < i + NSLOT:
                    wload(wstate["next_load"])
                    wstate["next_load"] += 1
                out = []
                for n in range(len(specs)):
                    assert wlist[i + n][1] == specs[n][1]
                    s = (i + n) % NSLOT
                    out.append((ring[s], B_ring[s]))
                    slot_of[id(B_ring[s])] = s
                return out

            def wget(src, nf=8):
                return wget_group([(src, nf)])[0]

            def kview(w2d):
                return w2d.rearrange("(k p) n -> p k n", p=128)

            def fview(w2d):
                return w2d.rearrange("(f p) d -> p f d", p=128)

            def ld(eng, dst, src, buf, key):
                sch.op(eng, lambda e: e.dma_start(out=dst, in_=src), writes=[buf], dma_key=key)

            for tg in range(TG):
                sch.op("sp", lambda e, tg=tg: e.dma_start(out=xT[:, :, tg * TW:(tg + 1) * TW], in_=xT_d[:, :, tg * TW:(tg + 1) * TW]),
                       writes=[B_x[kc][tg] for kc in range(KC)], dma_key=("x", tg))
            ld("act", vecs[:, :], vecs_d[:, :], B_c["vecs"], "vecs")
            ld("act", consts[:, :, :], consts_d[:, :, :], B_c["consts"], "consts")
            ld("act", router[:, :, :], router_d[:, :, :], B_c["router"], "router")

            sch.op("dve", lambda e: e.memset(ones_m[:, :], 1.0 / D), writes=[B_c["ones_m"]])
            sch.op("dve", lambda e: e.memset(ones_1[:, :], 1.0), writes=[B_c["ones_1"]])
            B_c["eps"] = Buf("eps")
            sch.op("dve", lambda e: e.memset(eps_t[:, :], EPS), writes=[B_c["eps"]])

            def rsqrt_eps(out_ap, in_ap, reads, Bout):
                sch.op("act", lambda e: e.activation(out=out_ap, in_=in_ap, func=AF.Ln, bias=eps_t[0:out_ap.shape[0], :]),
                       reads=list(reads) + [B_c["eps"]], writes=[Bout])
                sch.op("act", lambda e: e.activation(out=out_ap, in_=out_ap, func=AF.Exp, scale=-0.5), writes=[Bout])
            sch.op("dve", lambda e: e.memset(blk_m[:, :], 0.0), writes=[B_c["blk_m"]])
            sch.op("dve", lambda e: e.memset(blk_m[0:64, 0:64], 1.0 / 64), writes=[B_c["blk_m"]])
            sch.op("dve", lambda e: e.memset(blk_m[64:128, 64:128], 1.0 / 64), writes=[B_c["blk_m"]])
            sch.op("dve", lambda e: e.tensor_copy(out=identb[:, :], in_=consts[:, 0, :]),
                   reads=[B_c["consts"]], writes=[B_c["identb"]])

            def rmsnorm(gcol, out_bf16=True, final=False, keep_rstd=None, tgs=range(TG)):
                for tg in tgs:
                    tsl = slice(tg * TW, (tg + 1) * TW)
                    pst, Bpst = get_ps()
                    for kc in range(KC):
                        sq, Bsq = get_tmp()
                        sch.op("act", lambda e, sq=sq, kc=kc, tsl=tsl: e.activation(
                            out=sq[:, :], in_=xT[:, kc, tsl], func=AF.Square),
                            reads=[B_x[kc][tg]], writes=[Bsq])
                        sch.op("pe", lambda e, sq=sq, kc=kc, pst=pst: e.matmul(
                            pst[:, :], ones_m[:, :], sq[:, :], start=(kc == 0), stop=(kc == KC - 1)),
                            reads=[Bsq, B_c["ones_m"]], writes=[Bpst])
                    rstd, Brstd = get_tmp()
                    rsqrt_eps(rstd[:, :], pst[:, :], [Bpst], Brstd)
                    if keep_rstd is not None:
                        keep_rstd(tg, rstd, Brstd)
                    for kc in range(KC):
                        if final:
                            sch.op("dve", lambda e, kc=kc, tsl=tsl, rstd=rstd: e.scalar_tensor_tensor(
                                out=xT[:, kc, tsl], in0=xT[:, kc, tsl], scalar=vecs[:, gcol + kc:gcol + kc + 1],
                                in1=rstd[:, :], op0=ALU.mult, op1=ALU.mult),
                                reads=[Brstd, B_c["vecs"]], writes=[B_x[kc][tg]])
                        else:
                            sch.op("dve", lambda e, kc=kc, tsl=tsl, rstd=rstd: e.scalar_tensor_tensor(
                                out=hT[:, kc, tsl], in0=xT[:, kc, tsl], scalar=vecs[:, gcol + kc:gcol + kc + 1],
                                in1=rstd[:, :], op0=ALU.mult, op1=ALU.mult),
                                reads=[Brstd, B_x[kc][tg], B_c["vecs"]], writes=[B_h[kc][tg]])

            def mm_group(pst, Bpst, lhs_fn, rhs_fn, nk, reads, cols=None):
                def f(e):
                    last = None
                    for k in range(nk):
                        o = pst[:, :] if cols is None else pst[:, cols]
                        last = e.matmul(o, lhs_fn(k), rhs_fn(k), start=(k == 0), stop=(k == nk - 1))
                    return last
                sch.op("pe", f, reads=reads, writes=[Bpst])

            pre_normed = {"v": False}
            out_evs = []

            def store_out(tg):
                ev = sch.op("sp", lambda e, tg=tg: e.dma_start(out=yT_d[:, :, tg * TW:(tg + 1) * TW],
                                                               in_=xT[:, :, tg * TW:(tg + 1) * TW]),
                            reads=[B_x[kc][tg] for kc in range(KC)], dma_key=("out", tg))
                out_evs.append(ev)

            for l in range(L):
                ld("act", sgu_gb[:, :, :], sgu_gb_d[:, l, :, :], B_c["sgu_gb"], "sgu_gb")
                ld("act", sgu_bb[:, :, :], sgu_bb_d[:, l, :, :], B_c["sgu_bb"], "sgu_bb")
                ld("act", sgu_wTf[:, :, :], sgu_wT_d[:, l, :, :], B_c["sgu_wTf"], "sgu_wTf")
                def mkw(e):
                    return e.tensor_tensor(out=sgu_wTb[:, :, :], in0=sgu_wTf[:, :, :],
                                           in1=consts[:, 1:2, :].to_broadcast([128, 4, 128]), op=ALU.mult)
                sch.op("dve", mkw, reads=[B_c["sgu_wTf"], B_c["consts"]], writes=[B_c["sgu_wTb"]])

                fence_bufs = [B_cph] + B_diag + B_u + [b for r_ in B_cp for b in r_] + [b for r_ in B_act for b in r_]
                for j in range(4):
                    sch.op("dve", lambda e, j=j: e.memset(cpad[:, j, 0:30], 0.0), writes=fence_bufs if j == 0 else [B_cph])

                if not pre_normed["v"]:
                    rmsnorm(v_nmix(l))
                pre_normed["v"] = False
                if stop_after == ("dbg_h", l):
                    for kc in range(KC):
                        for tg in range(TG):
                            sch.op("dve", lambda e, kc=kc, tg=tg: e.tensor_copy(out=xT[:, kc, tg * TW:(tg + 1) * TW],
                                                                                in_=hT[:, kc, tg * TW:(tg + 1) * TW]),
                                   reads=[B_h[kc][tg]], writes=[B_x[kc][tg]])
                    break
                win = kview(w_in_d[l])

                def c_diag(j):
                    di = j % 2
                    dgm, Bdg = diag[di], B_diag[di]
                    for k in range(CW):
                        c0 = v_cw(l) + j * CW + k
                        sch.op("dve", lambda e, dgm=dgm, k=k, c0=c0: e.tensor_scalar(
                            out=dgm[:, k, :], in0=identb[:, :], scalar1=vecs[:, c0:c0 + 1], scalar2=None, op0=ALU.mult),
                            reads=[B_c["identb"], B_c["vecs"]], writes=[Bdg] if k in (0, CW - 1) else [])

                c_diag(0)
                c_diag(1)

                for j in range(4):
                    (wa, Bwa), (wg_, Bwg) = wget_group([(win[:, :, j * 128:(j + 1) * 128], 8),
                                                        (win[:, :, 512 + j * 128:512 + (j + 1) * 128], 8)])
                    for tg in range(TG):
                        tsl = slice(tg * TW, (tg + 1) * TW)
                        hb = [B_h[kc][tg] for kc in range(KC)]
                        pa, Bpa = get_ps()
                        mm_group(pa, Bpa, lambda k, wa=wa: wa[:, k, :], lambda k, tsl=tsl: hT[:, k, tsl], KC, hb + [Bwa])
                        pg, Bpg = get_ps()
                        mm_group(pg, Bpg, lambda k, wg_=wg_: wg_[:, k, :], lambda k, tsl=tsl: hT[:, k, tsl], KC, hb + [Bwg])
                        sg, Bsg = get_tmp()
                        sch.op("act", lambda e, sg=sg, pg=pg: e.activation(out=sg[:, :], in_=pg[:, :], func=AF.Sigmoid),
                               reads=[Bpg], writes=[Bsg])
                        sch.op("dve", lambda e, sg=sg, pa=pa, j=j, tg=tg: e.tensor_tensor(
                            out=cpad[:, j, 30 + tg * TW:30 + (tg + 1) * TW], in0=pa[:, :], in1=sg[:, :], op=ALU.mult),
                            reads=[Bpa, Bsg, B_cph], writes=[B_cp[j][tg]])

                if stop_after == ("dbg_c", l):
                    for j in range(4):
                        for tg in range(TG):
                            sch.op("dve", lambda e, j=j, tg=tg: e.tensor_copy(out=xT[:, j, tg * TW:(tg + 1) * TW],
                                                                              in_=cpad[:, j, 30 + tg * TW:30 + (tg + 1) * TW]),
                                   reads=[B_cp[j][tg]], writes=[B_x[j][tg]])
                    break
                uv_p = wget_group([(win[:, :, 1024 + j * 128:1024 + (j + 1) * 128], 8) for j in range(8)])
                wu_p, wv_p = uv_p[0:4], uv_p[4:8]
                bstate = {}
                BENG = "pool"

                def b_stage1(tt):
                    tg, t4 = divmod(tt, 4)
                    tsl = slice(tg * TW, (tg + 1) * TW)
                    hb = [B_h[kc][tg] for kc in range(KC)]
                    if t4 == 0:
                        ui = nxt("u", 2)
                        ut, But = u_t[ui], B_u[ui]
                        bstate[("u", tg)] = (ut, But)
                        for j in range(4):
                            pu, Bpu = get_ps()
                            w_, Bw_ = wu_p[j]
                            mm_group(pu, Bpu, lambda k, w_=w_: w_[:, k, :], lambda k, tsl=tsl: hT[:, k, tsl], KC, hb + [Bw_])
                            sch.op("act", lambda e, pu=pu, ut=ut, j=j: e.activation(out=ut[:, j, :], in_=pu[:, :], func=AF.Gelu),
                                   reads=[Bpu], writes=[But])
                    ksl = slice(tt * 128, (tt + 1) * 128)
                    pv, Bpv = get_ps()
                    vs0 = 0 if dry else slot_of[id(wv_p[0][1])]
                    if not dry:
                        assert [slot_of[id(wv_p[hd][1])] for hd in range(4)] == [vs0 + hd for hd in range(4)]
                    def fv(e, pv=pv, ksl=ksl, vs0=vs0):
                        last = None
                        for k in range(KC):
                            last = e.matmul(pv[:, :], hT[:, k, ksl], ring_all[:, vs0:vs0 + 4, k, :],
                                            start=(k == 0), stop=(k == KC - 1))
                        return last
                    sch.op("pe", fv, reads=hb + [wv_p[hd][1] for hd in range(4)], writes=[Bpv])
                    gv, Bgv = get_tmp()
                    sch.op("act", lambda e, gv=gv, pv=pv: e.activation(out=gv[:, :], in_=pv[:, :], func=AF.Gelu),
                           reads=[Bpv], writes=[Bgv])
                    sq, Bsq = get_tmp()
                    sch.op("dve", lambda e, sq=sq, gv=gv: e.tensor_tensor(out=sq[:, :], in0=gv[:, :], in1=gv[:, :], op=ALU.mult),
                           reads=[Bgv], writes=[Bsq])
                    si = tt % 8
                    Bm, Bs4 = B_c["m1"], B_c["st4"]
                    gv3 = gv[:, :].rearrange("p (h c) -> p h c", h=4)
                    sq3 = sq[:, :].rearrange("p (h c) -> p h c", h=4)
                    sch.op("dve", lambda e, gv3=gv3: e.tensor_reduce(out=m1[:, 0:4], in_=gv3, axis=AX.X, op=ALU.add),
                           reads=[Bgv], writes=[Bm])
                    sch.op("dve", lambda e, sq3=sq3: e.tensor_reduce(out=m1[:, 4:8], in_=sq3, axis=AX.X, op=ALU.add),
                           reads=[Bsq], writes=[Bm])
                    sch.op(BENG, lambda e: e.tensor_scalar(out=m1[:, 0:8], in0=m1[:, 0:8], scalar1=1.0 / 128, scalar2=None, op0=ALU.mult),
                           writes=[Bm])
                    sch.op(BENG, lambda e: e.tensor_tensor(out=m1[:, 8:12], in0=m1[:, 0:4], in1=m1[:, 0:4], op=ALU.mult),
                           writes=[Bm])
                    sch.op(BENG, lambda e: e.tensor_tensor(out=m1[:, 4:8], in0=m1[:, 4:8], in1=m1[:, 8:12], op=ALU.subtract),
                           writes=[Bm])
                    rsqrt_eps(st4[:, si, :], m1[:, 4:8], [Bm], Bs4)
                    for hd in range(4):
                        sch.op("dve", lambda e, hd=hd, sq=sq, gv=gv, si=si: e.tensor_scalar(
                            out=sq[:, hd * 128:(hd + 1) * 128], in0=gv[:, hd * 128:(hd + 1) * 128],
                            scalar1=m1[:, hd:hd + 1], scalar2=st4[:, si, hd:hd + 1], op0=ALU.subtract, op1=ALU.mult),
                            reads=[Bgv, Bm, Bs4], writes=[Bsq])
                    vi = nxt("vn", 4)
                    vn, Bvn = vn_t[vi], B_vn[vi]
                    sch.op(BENG, lambda e, sq=sq: e.tensor_tensor(out=sq[:, :], in0=sq[:, :], in1=sgu_gb[:, 0, :], op=ALU.mult),
                           reads=[B_c["sgu_gb"]], writes=[Bsq])
                    sch.op(BENG, lambda e, sq=sq, vn=vn: e.tensor_tensor(out=vn[:, :], in0=sq[:, :], in1=sgu_gb[:, 1, :], op=ALU.add),
                           reads=[Bsq, B_c["sgu_gb"]], writes=[Bvn])
                    bstate[("vn", tt)] = (vn, Bvn)

                def b_stage2(tt):
                    tg, t4 = divmod(tt, 4)
                    ksl = slice(tt * 128, (tt + 1) * 128)
                    ut, But = bstate[("u", tg)]
                    vn, Bvn = bstate[("vn", tt)]
                    pp, Bpp = get_ps()
                    def fsp(e, pp=pp, vn=vn):
                        last = None
                        for hd in range(4):
                            last = e.matmul(pp[:, hd * 128:(hd + 1) * 128], vn[:, hd * 128:(hd + 1) * 128],
                                            sgu_wTb[:, hd, :], start=True, stop=True)
                        return last
                    sch.op("pe", fsp, reads=[Bvn, B_c["sgu_wTb"]], writes=[Bpp])
                    tb, Btb = get_tmp()
                    sch.op("dve", lambda e, pp=pp, tb=tb: e.tensor_tensor(
                        out=tb[:, :], in0=pp[:, :], in1=sgu_bb[:, :, :].rearrange("p h t -> p (h t)"), op=ALU.add),
                        reads=[Bpp, B_c["sgu_bb"]], writes=[Btb])
                    sch.op("dve", lambda e, tb=tb, ut=ut, t4=t4, ksl=ksl: e.tensor_tensor(
                        out=hT[:, 4:8, ksl], in0=tb[:, :].rearrange("p (h t) -> p h t", h=4),
                        in1=ut[:, :, t4 * 128:(t4 + 1) * 128], op=ALU.mult),
                        reads=[Btb, But], writes=[B_y[tt]], war=[B_h[kc][tg] for kc in range(4, 8)])


                cstate = {}

                def c_stage1(j, tg):
                    di = j % 2
                    dgm, Bdg = diag[di], B_diag[di]
                    pc, Bpc = get_ps()
                    rd = [Bdg, B_cp[j][tg], B_cph] + ([B_cp[j][tg - 1]] if tg > 0 else [])
                    mm_group(pc, Bpc, lambda k, dgm=dgm: dgm[:, k, :],
                             lambda k, j=j, tg=tg: cpad[:, j, tg * TW + k: tg * TW + k + TW], CW, rd)
                    y, By = get_tmp()
                    ysq, Bysq = get_tmp()
                    cb = vecs[:, v_cb(l) + j: v_cb(l) + j + 1]
                    sch.op("act", lambda e, y=y, pc=pc, cb=cb: e.activation(out=y[:, :], in_=pc[:, :], func=AF.Identity, bias=cb),
                           reads=[Bpc, B_c["vecs"]], writes=[By])
                    sch.op("act", lambda e, ysq=ysq, y=y: e.activation(out=ysq[:, :], in_=y[:, :], func=AF.Square),
                           reads=[By], writes=[Bysq])
                    cstate[(j, tg)] = (y, By, ysq, Bysq)

                def c_stage2(j, tg):
                    y, By, ysq, Bysq = cstate[(j, tg)]
                    pm1, Bpm1 = get_ps()
                    pm2, Bpm2 = get_ps()
                    sch.op("pe", lambda e, pm1=pm1, y=y: e.matmul(pm1[:, :], blk_m[:, :], y[:, :], start=True, stop=True),
                           reads=[By, B_c["blk_m"]], writes=[Bpm1])
                    sch.op("pe", lambda e, pm2=pm2, ysq=ysq: e.matmul(pm2[:, :], blk_m[:, :], ysq[:, :], start=True, stop=True),
                           reads=[Bysq, B_c["blk_m"]], writes=[Bpm2])
                    sch.op("act", lambda e, ysq=ysq, pm1=pm1: e.activation(out=ysq[:, :], in_=pm1[:, :], func=AF.Square),
                           reads=[Bpm1], writes=[Bysq])
                    sch.op("dve", lambda e, ysq=ysq, pm2=pm2: e.tensor_tensor(out=ysq[:, :], in0=pm2[:, :], in1=ysq[:, :], op=ALU.subtract),
                           reads=[Bpm2], writes=[Bysq])
                    sch.op("dve", lambda e, y=y, pm1=pm1: e.tensor_tensor(out=y[:, :], in0=y[:, :], in1=pm1[:, :], op=ALU.subtract),
                           reads=[Bpm1], writes=[By])
                    rsqrt_eps(ysq[:, :], ysq[:, :], [], Bysq)
                    sch.op("dve", lambda e, y=y, ysq=ysq: e.tensor_tensor(out=y[:, :], in0=y[:, :], in1=ysq[:, :], op=ALU.mult),
                           reads=[Bysq], writes=[By])
                    ng = vecs[:, v_cng(l) + j: v_cng(l) + j + 1]
                    nb = vecs[:, v_cnb(l) + j: v_cnb(l) + j + 1]
                    sch.op("act", lambda e, y=y, j=j, tg=tg, ng=ng, nb=nb: e.activation(
                        out=cpad[:, j, 30 + tg * TW:30 + (tg + 1) * TW], in_=y[:, :], func=AF.Silu, bias=nb, scale=ng),
                        reads=[By, B_c["vecs"]], writes=[B_cp[j][tg]])

                ctiles = [(j, tg) for j in range(4) for tg in range(TG - 1, -1, -1)]
                b_seq = []
                BD = 3
                for tt in range(16):
                    step = [lambda tt=tt: b_stage1(tt)]
                    if tt >= BD:
                        step.append(lambda tt=tt: b_stage2(tt - BD))
                    b_seq.append(step)
                for tt in range(16 - BD, 16):
                    b_seq.append([lambda tt=tt: b_stage2(tt)])
                c_seq = [[]]
                for i, (j, tg) in enumerate(ctiles):
                    step = [lambda j=j, tg=tg: c_stage1(j, tg)]
                    if i >= 1:
                        pj, ptg = ctiles[i - 1]
                        step.append(lambda pj=pj, ptg=ptg: c_stage2(pj, ptg))
                        if ptg == 0 and pj + 2 < 4:
                            step.append(lambda pj=pj: c_diag(pj + 2))
                    c_seq.append(step)
                c_seq.append([lambda: c_stage2(*ctiles[-1])])
                for i in range(max(len(b_seq), len(c_seq))):
                    if i < len(b_seq):
                        for f_ in b_seq[i]:
                            f_()
                    if i < len(c_seq):
                        for f_ in c_seq[i]:
                            f_()

                wout = kview(w_out_d[l])

                def phase_d(tgs):
                    for dc in range(KC):
                        wo, Bwo = wget(wout[:, :, dc * 128:(dc + 1) * 128])
                        for tg in tgs:
                            tsl = slice(tg * TW, (tg + 1) * TW)
                            po, Bpo = get_ps()
                            mm_group(po, Bpo, lambda k, wo=wo: wo[:, k, :],
                                     lambda k, tsl=tsl, tg=tg: (cpad[:, k, 30 + tg * TW:30 + (tg + 1) * TW] if k < 4 else hT[:, k, tsl]), KC,
                                     [B_cp[k][tg] for k in range(4)] + [B_h[kc][tg] for kc in range(4, KC)]
                                     + [B_y[tg * 4 + q] for q in range(4)] + [Bwo])
                            sch.op("dve", lambda e, po=po, dc=dc, tsl=tsl: e.tensor_tensor(
                                out=xT[:, dc, tsl], in0=xT[:, dc, tsl], in1=po[:, :], op=ALU.add),
                                reads=[Bpo], writes=[B_x[dc][tg]])

                if stop_after == ("mixer", l):
                    phase_d(range(TG))
                    break

                moe = (l % 2 == 1)
                keep_fn = None
                if moe:
                    for kc in range(KC):
                        c0 = v_nffn(l) + kc
                        sch.op("dve", lambda e, kc=kc, c0=c0: e.tensor_scalar(
                            out=rg[:, kc, :], in0=router[:, kc, :], scalar1=vecs[:, c0:c0 + 1], scalar2=None, op0=ALU.mult),
                            reads=[B_c["router"], B_c["vecs"]], writes=[B_c["rg"]])
                    def keep8(tg, rstd, Brstd):
                        tsl = slice(tg * TW, (tg + 1) * TW)
                        pl, Bpl = get_ps()
                        def f(e, pl=pl, tsl=tsl):
                            last = None
                            for k in range(KC):
                                last = e.matmul(pl[0:8, :], rg[:, k, :], xT[:, k, tsl], start=(k == 0), stop=(k == KC - 1))
                            return last
                        sch.op("pe", f, reads=[B_x[kc][tg] for kc in range(KC)] + [B_c["rg"]], writes=[Bpl])
                        sch.op("dve", lambda e, pl=pl, rstd=rstd, tsl=tsl: e.tensor_tensor(
                            out=logT[0:8, tsl], in0=pl[0:8, :], in1=rstd[0:8, :], op=ALU.mult),
                            reads=[Bpl, Brstd], writes=[B_c["logT"]])
                    keep_fn = keep8
                for tgs_ in TSPLIT:
                    phase_d(tgs_)
                    rmsnorm(v_nffn(l), keep_rstd=keep_fn, tgs=tgs_)
                if moe:
                    pt, Bpt = get_ps()
                    def ftr(e, pt=pt):
                        last = None
                        for tt in range(16):
                            last = e.matmul(pt[:, tt * 8:(tt + 1) * 8], logT[0:8, tt * 128:(tt + 1) * 128],
                                            consts[0:8, 0, 0:8], start=True, stop=True)
                        return last
                    sch.op("pe", ftr, reads=[B_c["logT"], B_c["consts"]], writes=[Bpt])
                    Bc = B_c
                    Lv = Ltok[:, :, :]
                    bc3 = lambda t: t[:, :].unsqueeze(2).to_broadcast([128, 16, NE])
                    sch.op("dve", lambda e, pt=pt: e.tensor_copy(out=Lv, in_=pt[:, 0:128].rearrange("p (t e) -> p t e", e=NE)),
                           reads=[Bpt], writes=[Bc["Ltok"]])
                    sch.op("dve", lambda e: e.tensor_reduce(out=m1[:, :], in_=Lv, axis=AX.X, op=ALU.max),
                           reads=[Bc["Ltok"]], writes=[Bc["m1"]])
                    sch.op("dve", lambda e: e.tensor_tensor(out=eq1[:, :, :], in0=Lv, in1=bc3(m1), op=ALU.is_equal),
                           reads=[Bc["Ltok"], Bc["m1"]], writes=[Bc["eq1"]])
                    sch.op("dve", lambda e: e.scalar_tensor_tensor(out=L2tok[:, :, :], in0=eq1[:, :, :], scalar=-1e30, in1=Lv,
                                                                   op0=ALU.mult, op1=ALU.add),
                           reads=[Bc["Ltok"], Bc["eq1"]], writes=[Bc["L2tok"]])
                    sch.op("dve", lambda e: e.tensor_reduce(out=m2[:, :], in_=L2tok[:, :, :], axis=AX.X, op=ALU.max),
                           reads=[Bc["L2tok"]], writes=[Bc["m2"]])
                    sch.op("dve", lambda e: e.tensor_tensor(out=eq2[:, :, :], in0=L2tok[:, :, :], in1=bc3(m2), op=ALU.is_equal),
                           reads=[Bc["L2tok"], Bc["m2"]], writes=[Bc["eq2"]])
                    sch.op("dve", lambda e: e.tensor_tensor(out=g2[:, :], in0=m1[:, :], in1=m2[:, :], op=ALU.subtract),
                           reads=[Bc["m1"], Bc["m2"]], writes=[Bc["g2"]])
                    sch.op("act", lambda e: e.activation(out=g1[:, :], in_=g2[:, :], func=AF.Sigmoid),
                           reads=[Bc["g2"]], writes=[Bc["g1"]])
                    sch.op("act", lambda e: e.activation(out=m2[:, :], in_=g2[:, :], func=AF.Sigmoid, scale=-1.0),
                           reads=[Bc["g2"]], writes=[Bc["m2"]])
                    sch.op("dve", lambda e: e.tensor_tensor(out=eq1[:, :, :], in0=eq1[:, :, :], in1=bc3(g1), op=ALU.mult),
                           reads=[Bc["g1"]], writes=[Bc["eq1"]])
                    sch.op("dve", lambda e: e.tensor_tensor(out=eq2[:, :, :], in0=eq2[:, :, :], in1=bc3(m2), op=ALU.mult),
                           reads=[Bc["m2"]], writes=[Bc["eq2"]])
                    sch.op("dve", lambda e: e.tensor_tensor(out=dg[:, :, :], in0=eq1[:, :, :], in1=eq2[:, :, :], op=ALU.add),
                           reads=[Bc["eq1"], Bc["eq2"]], writes=[Bc["dg"]])

                if stop_after == ("norm2", l):
                    break

                sch.op("dve", lambda e: e.memset(st4[:, 0, :], 0.0),
                       writes=[B_c["st4"], B_cph] + B_diag + B_u + [b for r_ in B_cp for b in r_] + [b for r_ in B_act for b in r_])
                n_exp = NE if moe else 1
                for ex in range(n_exp):
                    if moe:
                        wg_d, wu_d, wd_d = kview(moe_wg_d[0, ex]), kview(moe_wu_d[0, ex]), fview(moe_wd_d[0, ex])
                    else:
                        wg_d, wu_d, wd_d = kview(ffn_wg_d[0]), kview(ffn_wu_d[0]), fview(ffn_wd_d[0])
                    gate_ready = False
                    for (f0, nf) in FBLOCKS:
                        for fl in range(nf):
                            fc = f0 + fl
                            (wg_, Bwg), (wu_, Bwu) = wget_group([(wg_d[:, :, fc * 128:(fc + 1) * 128], 8),
                                                                 (wu_d[:, :, fc * 128:(fc + 1) * 128], 8)])
                            for tg in range(TG):
                                tsl = slice(tg * TW, (tg + 1) * TW)
                                hb = [B_h[kc][tg] for kc in range(KC)]
                                pg, Bpg = get_ps()
                                mm_group(pg, Bpg, lambda k, wg_=wg_: wg_[:, k, :], lambda k, tsl=tsl: hT[:, k, tsl], KC, hb + [Bwg])
                                pu, Bpu = get_ps()
                                mm_group(pu, Bpu, lambda k, wu_=wu_: wu_[:, k, :], lambda k, tsl=tsl: hT[:, k, tsl], KC, hb + [Bwu])
                                sg, Bsg = get_tmp()
                                sch.op("act", lambda e, sg=sg, pg=pg: e.activation(out=sg[:, :], in_=pg[:, :], func=AF.Silu),
                                       reads=[Bpg], writes=[Bsg])
                                sch.op("dve", lambda e, sg=sg, pu=pu, fl=fl, tsl=tsl: e.tensor_tensor(
                                    out=act[:, fl, tsl], in0=pu[:, :], in1=sg[:, :], op=ALU.mult),
                                    reads=[Bpu, Bsg], writes=[B_act[fl][tg]])
                            if moe and not gate_ready and fl == 1:
                                gate_ready = True
                                for tg in range(TG):
                                    pgt, Bpgt = get_ps()
                                    for t4 in range(4):
                                        tt = tg * 4 + t4
                                        gi = nxt("gd", 4)
                                        sch.op("dve", lambda e, gi=gi, tt=tt, ex=ex: e.tensor_scalar(
                                            out=gdiag[gi][:, :], in0=consts[:, 0, :], scalar1=dg[:, tt, ex:ex + 1],
                                            scalar2=None, op0=ALU.mult),
                                            reads=[B_c["dg"], B_c["consts"]], writes=[B_gd[gi]])
                                        sch.op("pe", lambda e, gi=gi, pgt=pgt, t4=t4: e.matmul(
                                            pgt[:, t4 * 128:(t4 + 1) * 128], ones_1[:, :], gdiag[gi][:, :], start=True, stop=True),
                                            reads=[B_gd[gi], B_c["ones_1"]], writes=[Bpgt])
                                    sch.op("act", lambda e, pgt=pgt, tg=tg: e.activation(
                                        out=gate_bc[:, tg * TW:(tg + 1) * TW], in_=pgt[:, :], func=AF.Identity),
                                        reads=[Bpgt], writes=[B_c["gate_bc"]])
                        def down_proj(tgs, f0=f0, nf=nf, wd_d=wd_d):
                            for dc in range(KC):
                                wd_, Bwd = wget(wd_d[:, f0:f0 + nf, dc * 128:(dc + 1) * 128], nf)
                                for tg in tgs:
                                    tsl = slice(tg * TW, (tg + 1) * TW)
                                    po, Bpo = get_ps()
                                    mm_group(po, Bpo, lambda k, wd_=wd_: wd_[:, k, :], lambda k, tsl=tsl: act[:, k, tsl], nf,
                                             [B_act[k][tg] for k in range(nf)] + [Bwd])
                                    if moe:
                                        tb, Btb = get_tmp()
                                        sch.op("dve", lambda e, tb=tb, po=po, tsl=tsl: e.tensor_tensor(
                                            out=tb[:, :], in0=po[:, :], in1=gate_bc[:, tsl], op=ALU.mult),
                                            reads=[Bpo, B_c["gate_bc"]], writes=[Btb])
                                        sch.op("dve", lambda e, tb=tb, dc=dc, tsl=tsl: e.tensor_tensor(
                                            out=xT[:, dc, tsl], in0=xT[:, dc, tsl], in1=tb[:, :], op=ALU.add),
                                            reads=[Btb], writes=[B_x[dc][tg]])
                                    else:
                                        sch.op("dve", lambda e, po=po, dc=dc, tsl=tsl: e.tensor_tensor(
                                            out=xT[:, dc, tsl], in0=xT[:, dc, tsl], in1=po[:, :], op=ALU.add),
                                            reads=[Bpo], writes=[B_x[dc][tg]])

                        last_block = (ex == n_exp - 1 and f0 == FBLOCKS[-1][0])
                        if last_block and stop_after is None:
                            for tgs_ in TSPLIT:
                                down_proj(tgs_)
                                if l + 1 < L:
                                    rmsnorm(v_nmix(l + 1), tgs=tgs_)
                                else:
                                    rmsnorm(V_NFIN, final=True, tgs=tgs_)
                                    for tg in tgs_:
                                        store_out(tg)
                            pre_normed["v"] = True
                        else:
                            down_proj(range(TG))
                    if stop_after == ("expert", l, ex):
                        break
                if stop_after is not None and stop_after[0] in ("ffn", "expert") and stop_after[1] == l:
                    break

            if stop_after is None:
                if not pre_normed["v"]:
                    rmsnorm(V_NFIN, final=True)
                    for tg in range(TG):
                        store_out(tg)
            else:
                for tg in range(TG):
                    store_out(tg)
            if not dry:
                for ev in out_evs:
                    b = Buf()
                    b.w = ev
                    sch.op("sp", lambda e: e.nop(), reads=[b])

        build_program.last_counts = dict(sch.cnt)
        sem = {}
        for e in Sched.ENGS:
            sem[e] = es.enter_context(nc.semaphore(f"s_{e}"))
        for n_, key in enumerate(sch.dma_cnt):
            sem[key] = es.enter_context(nc.semaphore(f"d_{n_}"))
        block = es.enter_context(nc.Block())

        def run(handle, items):
            for waits, fn, ev in items:
                for k, v in waits:
                    handle.wait_ge(sem[k], v)
                inst = fn(handle)
                if ev is not None:
                    inst.then_inc(sem[ev[0]], 16 if isinstance(ev[0], tuple) else 1)

        @block.tensor
        def _(t):
            run(t, sch.q["pe"])

        @block.scalar
        def _(a):
            run(a, sch.q["act"])

        @block.vector
        def _(v):
            run(v, sch.q["dve"])

        @block.gpsimd
        def _(g):
            run(g, sch.q["pool"])

        @block.sync
        def _(s):
            run(s, sch.q["sp"])
    return nc


def prep_inputs(inp):
    f = lambda a: np.ascontiguousarray(np.asarray(a, dtype=np.float32))
    x = f(inp["x"])
    B = x.shape[0]
    xT = [np.ascontiguousarray(x[b].T.reshape(KC, 128, S).transpose(1, 0, 2)) for b in range(B)]
    vecs = np.zeros((128, NV), np.float32)
    def cols8(v):
        return np.asarray(v, np.float32).reshape(8, 128).T
    def cols4(v):
        return np.asarray(v, np.float32).reshape(4, 128).T
    for l in range(L):
        vecs[:, v_nmix(l):v_nmix(l) + 8] = cols8(inp["norm_mix"][l])
        vecs[:, v_nffn(l):v_nffn(l) + 8] = cols8(inp["norm_ffn"][l])
        vecs[:, v_cb(l):v_cb(l) + 4] = cols4(inp["conv_b"][l])
        vecs[:, v_cng(l):v_cng(l) + 4] = cols4(inp["conv_ng"][l])
        vecs[:, v_cnb(l):v_cnb(l) + 4] = cols4(inp["conv_nb"][l])
        cw = np.asarray(inp["conv_w"][l], np.float32).reshape(CW, 4, 128)
        vecs[:, v_cw(l):v_cw(l) + 4 * CW] = cw.transpose(2, 1, 0).reshape(128, 4 * CW)
    vecs[:, V_NFIN:V_NFIN + 8] = cols8(inp["norm_final"])
    sgu_gb = np.empty((128, L, 2, 512), np.float32)
    sgu_gb[:, :, 0, :] = np.asarray(inp["sgu_ng"], np.float32)[None]
    sgu_gb[:, :, 1, :] = np.asarray(inp["sgu_nb"], np.float32)[None]
    sgu_bb = np.ascontiguousarray(np.broadcast_to(np.asarray(inp["sgu_b"], np.float32)[None], (128, L, 4, 128)))
    sgu_wT = np.ascontiguousarray(np.asarray(inp["sgu_w"], np.float32).transpose(3, 0, 1, 2))
    consts = np.zeros((128, 2, 128), np.float32)
    consts[:, 0, :] = np.eye(128, dtype=np.float32)
    consts[:, 1, :] = np.triu(np.ones((128, 128), np.float32))
    router = np.ascontiguousarray(np.asarray(inp["moe_router"], np.float32)[0].reshape(KC, 128, NE).transpose(1, 0, 2))
    shared = dict(vecs=vecs, sgu_gb=sgu_gb, sgu_bb=sgu_bb, sgu_wT=sgu_wT, consts=consts, router=router,
                  w_in=f(inp["w_in"]), w_out=f(inp["w_out"]), ffn_wg=f(inp["ffn_wg"]), ffn_wu=f(inp["ffn_wu"]),
                  ffn_wd=f(inp["ffn_wd"]), moe_wg=f(inp["moe_wg"]), moe_wu=f(inp["moe_wu"]), moe_wd=f(inp["moe_wd"]))
    return xT, shared


def unpack_out(yT):
    return np.ascontiguousarray(yT.transpose(1, 0, 2).reshape(D, S).T)


_NC_CACHE = {}


def kernel(**inputs):
    xT, shared = prep_inputs(inputs)
    B = len(xT)
    if "nc" not in _NC_CACHE:
        _NC_CACHE["nc"] = build_program()
    nc = _NC_CACHE["nc"]
    in_maps = [dict(shared, xT=xT[b]) for b in range(B)]
    res = run_bass_kernel_spmd(nc, in_maps, core_ids=list(range(B)))
    out = np.stack([unpack_out(res.results[b]["yT"]) for b in range(B)], axis=0)
    return out.astype(np.float32)
```
